# Optimizing a Trainium2 kernel written in Bass

```python
import math
import jax
import jax.numpy as jnp
from jax import lax
import numpy as np

D_MODEL = 2048
BATCH = 8
SEQ = 2048
DEPTH = 2

MEM_LEN = 256
DA_QK_DIM = 64
DA_V_DIM = 2 * DA_QK_DIM
DA_HEADS = D_MODEL // (4 * DA_V_DIM)
DA_QK_WIDTH = DA_HEADS * 2 * DA_QK_DIM
DA_WIDTH = DA_HEADS * DA_V_DIM
Q_BLOCK = 128
ROPE_THETA = 10000.0
ML_DIM = 128
ML_HEADS = D_MODEL // (4 * ML_DIM)
ML_WIDTH = ML_HEADS * ML_DIM
ML_CHUNK = 64
GD_DIM = 128
GD_HEADS = D_MODEL // (2 * GD_DIM)
GD_WIDTH = GD_HEADS * GD_DIM
GD_CHUNK = 64
CONV_W = 4
MIX_WIDTH = DA_WIDTH + ML_WIDTH + GD_WIDTH
IN_SIZES = (DA_QK_WIDTH, DA_QK_WIDTH, DA_WIDTH,
            ML_WIDTH, ML_WIDTH, ML_WIDTH, ML_WIDTH, ML_HEADS, ML_HEADS,
            3 * GD_WIDTH, GD_WIDTH, GD_HEADS, GD_HEADS)
IN_WIDTH = sum(IN_SIZES)
XA_HEADS = 4
XA_DIM = D_MODEL // XA_HEADS
N_EXPERTS = 16
N_GROUPS = 4
GROUP_SIZE = N_EXPERTS // N_GROUPS
TOP_GROUPS = 1
TOP_K = 2
D_FF = D_MODEL // 2

kernel_name = 'hybrid_diffattn_mlstm_gdn_moe'

F32 = jnp.float32


def layer_norm(x, g, b, eps=1e-5):
    xf = x.astype(F32)
    mu = jnp.mean(xf, -1, keepdims=True)
    var = jnp.mean(jnp.square(xf - mu), -1, keepdims=True)
    return ((xf - mu) * lax.rsqrt(var + eps) * g + b).astype(x.dtype)


def rms_norm(x, g, eps=1e-6):
    xf = x.astype(F32)
    return (xf * lax.rsqrt(jnp.mean(xf * xf, -1, keepdims=True) + eps) * g).astype(x.dtype)


def l2_norm(x, eps=1e-6):
    return x * lax.rsqrt(jnp.sum(x * x, -1, keepdims=True) + eps)


def rotary_tables(T):
    inv = 1.0 / (ROPE_THETA ** (jnp.arange(0, DA_QK_DIM, 2, dtype=F32) / DA_QK_DIM))
    ang = jnp.arange(T, dtype=F32)[:, None] * inv[None, :]
    return jnp.cos(ang), jnp.sin(ang)


def apply_rotary(t, cos, sin):
    t1, t2 = jnp.split(t, 2, axis=-1)
    return jnp.concatenate([t1 * cos - t2 * sin, t1 * sin + t2 * cos], -1).astype(t.dtype)


def split_columns(proj):
    offs, acc = [], 0
    for s in IN_SIZES[:-1]:
        acc += s
        offs.append(acc)
    return jnp.split(proj, offs, axis=-1)


def to_chunks(t, L):
    B, T, H, D = t.shape
    return t.reshape(B, T // L, L, H, D).transpose(1, 0, 3, 2, 4)


def gate_chunks(g, L):
    B, T, H = g.shape
    return g.reshape(B, T // L, L, H).transpose(1, 0, 3, 2)


def from_chunks(t):
    NC, B, H, L, D = t.shape
    return t.transpose(1, 0, 3, 2, 4).reshape(B, NC * L, H, D)


def diff_attention(q, k, v, lam, norm_g, lambda_init, cos, sin):
    B, T, _ = q.shape
    q = q.reshape(B, T, DA_HEADS, 2, DA_QK_DIM).transpose(0, 2, 3, 1, 4)
    k = k.reshape(B, T, DA_HEADS, 2, DA_QK_DIM).transpose(0, 2, 3, 1, 4)
    v = v.reshape(B, T, DA_HEADS, DA_V_DIM).transpose(0, 2, 1, 3)
    q = apply_rotary(q, cos, sin) * (DA_QK_DIM ** -0.5)
    k = apply_rotary(k, cos, sin)
    lamf = lam.astype(F32)
    lam_full = (jnp.exp(jnp.sum(lamf[0] * lamf[1])) - jnp.exp(jnp.sum(lamf[2] * lamf[3]))
                + lambda_init)
    outs = []
    for qs in range(0, T, Q_BLOCK):
        ke = qs + Q_BLOCK
        s = jnp.einsum('bhcqd,bhckd->bhcqk', q[:, :, :, qs:ke], k[:, :, :, :ke]).astype(F32)
        causal = jnp.arange(qs, ke)[:, None] >= jnp.arange(ke)[None, :]
        p = jax.nn.softmax(jnp.where(causal, s, -jnp.inf), axis=-1)
        a = (p[:, :, 0] - lam_full * p[:, :, 1]).astype(v.dtype)
        outs.append(jnp.einsum('bhqk,bhkd->bhqd', a, v[:, :, :ke]))
    o = jnp.concatenate(outs, axis=2)
    o = rms_norm(o, norm_g) * (1.0 - lambda_init)
    return o.transpose(0, 2, 1, 3).reshape(B, T, DA_WIDTH)


def mlstm(q, k, v, o_pre, i_pre, f_pre, gate_b, norm_g):
    B, T, _ = q.shape
    H, D, L = ML_HEADS, ML_DIM, ML_CHUNK
    dtype = q.dtype
    qc = to_chunks(q.reshape(B, T, H, D).astype(F32), L)
    kc = to_chunks(k.reshape(B, T, H, D).astype(F32), L) * (D ** -0.5)
    vc = to_chunks(v.reshape(B, T, H, D).astype(F32), L)
    li = gate_chunks(i_pre.astype(F32) + gate_b[0], L)
    lf = gate_chunks(jax.nn.log_sigmoid(f_pre.astype(F32) + gate_b[1]), L)
    causal = jnp.tril(jnp.ones((L, L), bool))

    def step(carry, inp):
        C, n, m = carry
        qb, kb, vb, lib, lfb = inp
        b = jnp.cumsum(lfb, -1)
        logw = jnp.where(causal, b[..., :, None] - b[..., None, :] + lib[..., None, :], -jnp.inf)
        logp = b + m[..., None]
        m_row = jnp.maximum(logp, jnp.max(logw, -1))
        w = jnp.exp(logw - m_row[..., None])
        sp = jnp.exp(logp - m_row)
        qk = jnp.einsum('bhsd,bhjd->bhsj', qb, kb) * w
        num = (jnp.einsum('bhsj,bhje->bhse', qk, vb)
               + sp[..., None] * jnp.einsum('bhed,bhsd->bhse', C, qb))
        den = jnp.sum(qk, -1) + sp * jnp.einsum('bhd,bhsd->bhs', n, qb)
        h = num / jnp.maximum(jnp.abs(den), jnp.exp(-m_row))[..., None]
        bL = b[..., -1]
        logu = bL[..., None] - b + lib
        m_new = jnp.maximum(bL + m, jnp.max(logu, -1))
        u = jnp.exp(logu - m_new[..., None])
        decay = jnp.exp(bL + m - m_new)
        C = decay[..., None, None] * C + jnp.einsum('bhj,bhje,bhjd->bhed', u, vb, kb)
        n = decay[..., None] * n + jnp.einsum('bhj,bhjd->bhd', u, kb)
        return (C, n, m_new), h

    init = (jnp.zeros((B, H, D, D), F32), jnp.zeros((B, H, D), F32), jnp.zeros((B, H), F32))
    _, h = lax.scan(step, init, (qc, kc, vc, li, lf))
    h = rms_norm(from_chunks(h), norm_g)
    o = jax.nn.sigmoid(o_pre.astype(F32)).reshape(B, T, H, D)
    return (o * h).reshape(B, T, ML_WIDTH).astype(dtype)


def causal_conv_silu(x, w):
    C = x.shape[-1]
    y = lax.conv_general_dilated(x, w[:, None, :].astype(x.dtype), window_strides=(1,),
                                 padding=((CONV_W - 1, 0),),
                                 dimension_numbers=('NWC', 'WIO', 'NWC'),
                                 feature_group_count=C)
    return jax.nn.silu(y)


def gated_deltanet(qkv, z, a_pre, b_pre, conv_w, a_log, dt_bias, norm_g):
    B, T, _ = qkv.shape
    H, D, L = GD_HEADS, GD_DIM, GD_CHUNK
    dtype = qkv.dtype
    q, k, v = jnp.split(causal_conv_silu(qkv, conv_w).astype(F32), 3, axis=-1)
    q = to_chunks(l2_norm(q.reshape(B, T, H, D)) * (D ** -0.5), L)
    k = to_chunks(l2_norm(k.reshape(B, T, H, D)), L)
    v = to_chunks(v.reshape(B, T, H, D), L)
    beta = gate_chunks(jax.nn.sigmoid(b_pre.astype(F32)), L)
    g = gate_chunks(-jnp.exp(a_log.astype(F32)) * jax.nn.softplus(a_pre.astype(F32) + dt_bias), L)
    gc = jnp.cumsum(g, -1)
    tri = jnp.tril(jnp.ones((L, L), bool))
    strict = jnp.tril(jnp.ones((L, L), bool), -1)
    decay = jnp.exp(jnp.where(tri, gc[..., :, None] - gc[..., None, :], -jnp.inf))
    kb = k * beta[..., None]
    A = jnp.where(strict, jnp.einsum('nbhid,nbhjd->nbhij', kb, k) * decay, 0.0)
    rhs = jnp.concatenate([v * beta[..., None], kb * jnp.exp(gc)[..., None]], -1)
    uw = lax.linalg.triangular_solve(A + jnp.eye(L, dtype=F32), rhs, left_side=True, lower=True)
    u, w = uw[..., :D], uw[..., D:]
    attn = jnp.where(tri, jnp.einsum('nbhid,nbhjd->nbhij', q, k) * decay, 0.0)

    def step(S, inp):
        qi, ki, ui, wi, gi, ai = inp
        v_new = ui - jnp.einsum('bhld,bhde->bhle', wi, S)
        o = (jnp.einsum('bhld,bhde->bhle', qi * jnp.exp(gi)[..., None], S)
             + jnp.einsum('bhlj,bhje->bhle', ai, v_new))
        gl = gi[..., -1]
        S = (S * jnp.exp(gl)[..., None, None]
             + jnp.einsum('bhld,bhle->bhde', ki * jnp.exp(gl[..., None] - gi)[..., None], v_new))
        return S, o

    _, o = lax.scan(step, jnp.zeros((B, H, D, D), F32), (q, k, u, w, gc, attn))
    o = rms_norm(from_chunks(o), norm_g)
    zg = jax.nn.silu(z.astype(F32)).reshape(B, T, H, D)
    return (o * zg).reshape(B, T, GD_WIDTH).astype(dtype)


def hybrid_mixer(h, w_in, da_lambda, da_norm_g, ml_gate_b, ml_norm_g, gd_conv_w,
                 gd_a_log, gd_dt_bias, gd_norm_g, w_out, lambda_init, cos, sin):
    (da_q, da_k, da_v, ml_q, ml_k, ml_v, ml_o, ml_i, ml_f,
     gd_qkv, gd_z, gd_a, gd_b) = split_columns(h @ w_in)
    y_da = diff_attention(da_q, da_k, da_v, da_lambda, da_norm_g, lambda_init, cos, sin)
    y_ml = mlstm(ml_q, ml_k, ml_v, ml_o, ml_i, ml_f, ml_gate_b, ml_norm_g)
    y_gd = gated_deltanet(gd_qkv, gd_z, gd_a, gd_b, gd_conv_w, gd_a_log, gd_dt_bias, gd_norm_g)
    return jnp.concatenate([y_da, y_ml, y_gd], axis=-1) @ w_out


def memory_cross_attention(h, mem, wq, wkv, wo):
    B, T, _ = h.shape
    M = mem.shape[1]
    q = (h @ wq).reshape(B, T, XA_HEADS, XA_DIM)
    kv = (mem @ wkv).reshape(B, M, 2, XA_HEADS, XA_DIM)
    s = jnp.einsum('bthd,bmhd->bhtm', q, kv[:, :, 0]).astype(F32) * (XA_DIM ** -0.5)
    p = jax.nn.softmax(s, axis=-1).astype(h.dtype)
    o = jnp.einsum('bhtm,bmhd->bthd', p, kv[:, :, 1]).reshape(B, T, XA_HEADS * XA_DIM)
    return o @ wo


def grouped_moe(h, router_w, router_bias, w_in, w_out):
    B, T, D = h.shape
    t = h.reshape(B * T, D)
    scores = jax.nn.sigmoid((t @ router_w).astype(F32))
    sel = scores + router_bias.astype(F32)
    grp_score = jnp.sum(lax.top_k(sel.reshape(-1, N_GROUPS, GROUP_SIZE), 2)[0], -1)
    _, gi = lax.top_k(grp_score, TOP_GROUPS)
    gmask = jnp.any(gi[:, :, None] == jnp.arange(N_GROUPS)[None, None, :], axis=1)
    emask = jnp.repeat(gmask, GROUP_SIZE, axis=1)
    _, ei = lax.top_k(jnp.where(emask, sel, -jnp.inf), TOP_K)
    wsel = jnp.take_along_axis(scores, ei, axis=-1)
    wsel = wsel / jnp.sum(wsel, -1, keepdims=True)
    combine = jnp.sum(jax.nn.one_hot(ei, N_EXPERTS, dtype=F32) * wsel[..., None], axis=1)
    y = jnp.zeros_like(t)
    for e in range(N_EXPERTS):
        gate, up = jnp.split(t @ w_in[e], 2, axis=-1)
        y = y + combine[:, e:e + 1].astype(t.dtype) * ((jax.nn.silu(gate) * up) @ w_out[e])
    return y.reshape(B, T, D)


def setup_inputs(seed: int = 0) -> dict:
    key = jax.random.key(seed)
    ks = jax.random.split(key, 32)
    D = D_MODEL
    beta = (8 * DEPTH) ** -0.25

    def nrm(k, shape, s):
        return jax.random.normal(k, shape, F32) * s

    x = nrm(ks[0], (BATCH, SEQ, D), 1.0)
    mem = nrm(ks[1], (BATCH, MEM_LEN, D), 1.0)
    w_in = nrm(ks[2], (DEPTH, D, IN_WIDTH), D ** -0.5)
    da_lambda = nrm(ks[3], (DEPTH, 4, DA_QK_DIM), 0.1)
    da_norm_g = 1.0 + nrm(ks[4], (DEPTH, DA_V_DIM), 0.02)
    ml_gate_b = jnp.stack([nrm(ks[5], (DEPTH, ML_HEADS), 0.1),
                           3.0 + 3.0 * jax.random.uniform(ks[6], (DEPTH, ML_HEADS), F32)], axis=1)
    ml_norm_g = 1.0 + nrm(ks[7], (DEPTH, ML_DIM), 0.02)
    gd_conv_w = nrm(ks[8], (DEPTH, CONV_W, 3 * GD_WIDTH), CONV_W ** -0.5)
    gd_a_log = jnp.log(jax.random.uniform(ks[9], (DEPTH, GD_HEADS), F32, 1.0, 16.0))
    dt = jnp.exp(jax.random.uniform(ks[10], (DEPTH, GD_HEADS), F32,
                                    math.log(1e-3), math.log(1e-1)))
    gd_dt_bias = dt + jnp.log(-jnp.expm1(-dt))
    gd_norm_g = 1.0 + nrm(ks[11], (DEPTH, GD_DIM), 0.02)
    w_out = nrm(ks[12], (DEPTH, MIX_WIDTH, D), (MIX_WIDTH ** -0.5) * beta)
    xa_wq = nrm(ks[13], (DEPTH, D, XA_HEADS * XA_DIM), D ** -0.5)
    xa_wkv = nrm(ks[14], (DEPTH, D, 2 * XA_HEADS * XA_DIM), D ** -0.5)
    xa_wo = nrm(ks[15], (DEPTH, XA_HEADS * XA_DIM, D), ((XA_HEADS * XA_DIM) ** -0.5) * beta)
    router_w = nrm(ks[16], (D, N_EXPERTS), D ** -0.5)
    router_bias = nrm(ks[17], (N_EXPERTS,), 0.01)
    moe_w_in = nrm(ks[18], (DEPTH, N_EXPERTS, D, 2 * D_FF), D ** -0.5)
    moe_w_out = nrm(ks[19], (DEPTH, N_EXPERTS, D_FF, D), (D_FF ** -0.5) * beta)
    ln_g = 1.0 + nrm(ks[20], (DEPTH, 3, D), 0.02)
    ln_b = nrm(ks[21], (DEPTH, 3, D), 0.02)
    return {'x': x, 'mem': mem, 'w_in': w_in, 'da_lambda': da_lambda, 'da_norm_g': da_norm_g,
            'ml_gate_b': ml_gate_b, 'ml_norm_g': ml_norm_g, 'gd_conv_w': gd_conv_w,
            'gd_a_log': gd_a_log, 'gd_dt_bias': gd_dt_bias, 'gd_norm_g': gd_norm_g,
            'w_out': w_out, 'xa_wq': xa_wq, 'xa_wkv': xa_wkv, 'xa_wo': xa_wo,
            'router_w': router_w, 'router_bias': router_bias, 'moe_w_in': moe_w_in,
            'moe_w_out': moe_w_out, 'ln_g': ln_g, 'ln_b': ln_b}


def reference(x, mem, w_in, da_lambda, da_norm_g, ml_gate_b, ml_norm_g, gd_conv_w,
              gd_a_log, gd_dt_bias, gd_norm_g, w_out, xa_wq, xa_wkv, xa_wo,
              router_w, router_bias, moe_w_in, moe_w_out, ln_g, ln_b):
    T = x.shape[1]
    cos, sin = rotary_tables(T)
    alpha = (2 * DEPTH) ** 0.25
    h = x
    for l in range(DEPTH):
        lambda_init = 0.8 - 0.6 * math.exp(-0.3 * l)
        mix = hybrid_mixer(h, w_in[l], da_lambda[l], da_norm_g[l], ml_gate_b[l], ml_norm_g[l],
                           gd_conv_w[l], gd_a_log[l], gd_dt_bias[l], gd_norm_g[l], w_out[l],
                           lambda_init, cos, sin)
        h = layer_norm(alpha * h + mix, ln_g[l, 0], ln_b[l, 0])
        xa = memory_cross_attention(h, mem, xa_wq[l], xa_wkv[l], xa_wo[l])
        h = layer_norm(alpha * h + xa, ln_g[l, 1], ln_b[l, 1])
        ff = grouped_moe(h, router_w, router_bias, moe_w_in[l], moe_w_out[l])
        h = layer_norm(alpha * h + ff, ln_g[l, 2], ln_b[l, 2])
    return h
```

```python
import numpy as np
import concourse.bass as bass
import concourse.mybir as mybir
from concourse.bass_utils import run_bass_kernel_spmd

F32 = mybir.dt.float32
BF16 = mybir.dt.bfloat16
AF = mybir.ActivationFunctionType
ALU = mybir.AluOpType
AX = mybir.AxisListType

COMPUTE = ('pe', 'dve', 'act', 'pool')
NSLOT = 6


class Prog:
    def __init__(self, nc):
        self.nc = nc
        self.streams = {e: [] for e in ('pe', 'dve', 'act', 'pool', 'sp')}
        self.cnt = {e: 0 for e in COMPUTE}
        self.dcnt = {q: 0 for q in ('sp', 'act', 'pool')}
        self.waited = {}
        self.lastw = {}
        self.readers = {}
        self.nwaits = 0

    def _need(self, eng, tok):
        if tok[0] == 'c':
            key = (eng, 'c', tok[1])
            val = tok[2]
            if tok[1] == eng and eng == 'pe':
                return
        else:
            key = (eng, 'd', tok[1], tok[2])
            val = tok[3]
        if self.waited.get(key, 0) >= val:
            return
        self.waited[key] = val
        self.streams[eng].append(('wait', tok))
        self.nwaits += 1

    def _deps(self, eng, r, w):
        toks = []
        for k in list(r) + list(w):
            t = self.lastw.get(k)
            if t is not None:
                toks.append(t)
        for k in w:
            toks.extend(self.readers.get(k, ()))
        for t in toks:
            self._need(eng, t)

    def _commit(self, tok, r, w):
        for k in w:
            self.lastw[k] = tok
            self.readers[k] = []
        for k in r:
            if k in w:
                continue
            self.readers.setdefault(k, []).append(tok)
            if len(self.readers[k]) > 64:
                seen = {}
                for t in self.readers[k]:
                    kk = t[:2] if t[0] == 'c' else t[:3]
                    if kk not in seen or seen[kk][-1] < t[-1]:
                        seen[kk] = t
                self.readers[k] = list(seen.values())

    def op(self, eng, fn, r=(), w=()):
        self._deps(eng, r, w)
        self.cnt[eng] += 1
        tok = ('c', eng, self.cnt[eng])
        self.streams[eng].append(('ins', fn, tok))
        self._commit(tok, r, w)

    def dma(self, fn, r=(), w=(), q='sp'):
        self._deps(q, r, w)
        i = self.dcnt[q]
        self.dcnt[q] += 1
        tok = ('d', q, i % NSLOT, 16 * (i // NSLOT + 1))
        self.streams[q].append(('ins', fn, tok))
        self._commit(tok, r, w)

    def barrier(self):
        toks = [('c', e, self.cnt[e]) for e in COMPUTE if self.cnt[e] > 0]
        for q in self.dcnt:
            n = self.dcnt[q]
            for s in range(NSLOT):
                m = (n - s + NSLOT - 1) // NSLOT if n > s else 0
                if m > 0:
                    toks.append(('d', q, s, 16 * m))
        for e in self.streams:
            for t in toks:
                if t[0] == 'c' and t[1] == e:
                    continue
                self._need(e, t)
        self.lastw = {}
        self.readers = {}

    def emit(self, final_engine='sp'):
        nc = self.nc
        self.barrier()
        import contextlib
        with contextlib.ExitStack() as es:
            csem = {e: es.enter_context(nc.semaphore('c_' + e)) for e in COMPUTE}
            dsem = {(q, s): es.enter_context(nc.semaphore('d_%s%d' % (q, s)))
                    for q in self.dcnt for s in range(NSLOT)}
            block = es.enter_context(nc.Block())

            def run(engname, eh):
                for item in self.streams[engname]:
                    if item[0] == 'wait':
                        t = item[1]
                        if t[0] == 'c':
                            eh.wait_ge(csem[t[1]], t[2])
                        else:
                            eh.wait_ge(dsem[(t[1], t[2])], t[3])
                    else:
                        _, fn, t = item
                        ins = fn(eh)
                        if t[0] == 'c':
                            ins.then_inc(csem[t[1]], 1)
                        else:
                            ins.then_inc(dsem[(t[1], t[2])], 16)

            @block.sync
            def _(e):
                run('sp', e)

            @block.tensor
            def _(e):
                run('pe', e)

            @block.vector
            def _(e):
                run('dve', e)

            @block.scalar
            def _(e):
                run('act', e)

            @block.gpsimd
            def _(e):
                run('pool', e)


class SB:
    def __init__(self, pool_ap, nbytes):
        self.pool = pool_ap
        self.n = nbytes
        self.off = 0
        self.uid = 0

    def mark(self):
        return self.off

    def release(self, m):
        self.off = m

    def alloc(self, shape_free, dt, parts=128):
        esz = 2 if dt == BF16 else 4
        n = int(np.prod(shape_free))
        nb = (n * esz + 31) // 32 * 32
        assert self.off + nb <= self.n, ('SBUF overflow', self.off, nb, self.n)
        a = self.pool[0:parts, self.off // 4:(self.off + nb) // 4]
        self.off += nb
        self.maxoff = max(getattr(self, 'maxoff', 0), self.off)
        if dt != F32:
            a = a.bitcast(dt)
        a = a[:, 0:n]
        if len(shape_free) == 2:
            a = a.rearrange('p (a b) -> p a b', a=shape_free[0])
        elif len(shape_free) == 3:
            a = a.rearrange('p (a b c) -> p a b c', a=shape_free[0], b=shape_free[1])
        return a


T = 2048
D = 2048
NT = 16
KC = 16
DEPTH = 2
IN_W = 7704
ALPHA = (2 * DEPTH) ** 0.25
C_DAQ, C_DAK, C_DAV = 0, 512, 1024
C_MLQ, C_MLK, C_MLV, C_MLO, C_MLI, C_MLF = 1536, 2048, 2560, 3072, 3584, 3588
C_GDQKV, C_GDZ, C_GDA, C_GDB = 3592, 6664, 7688, 7696


class MK:
    def __init__(self, nc, P, sb, ps, dr):
        self.nc, self.P, self.sb, self.ps, self.dr = nc, P, sb, ps, dr
        self.bank_i = 0
        self.uid = 0

    def key(self, s):
        self.uid += 1
        return '%s#%d' % (s, self.uid)

    def bank(self):
        b = self.bank_i % 8
        self.bank_i += 1
        return self.ps[:, b * 512:(b + 1) * 512], 'ps%d' % b

    def consts(self):
        P, sb = self.P, self.sb
        self.ident = sb.alloc([128], F32)
        P.dma(lambda e: e.dma_start(out=self.ident, in_=self.dr['ident']), w=['ident'])
        self.m_ge = sb.alloc([128], F32)
        self.m_gt = sb.alloc([128], F32)
        self.m_lt = sb.alloc([128], F32)
        self.ones = sb.alloc([128], F32)
        self.m_ge_b = sb.alloc([128], BF16)
        self.ones_b = sb.alloc([128], BF16)
        for m, pat, cm, base in ((self.m_ge, 1, -1, 0), (self.m_gt, 1, -1, -1), (self.m_lt, -1, 1, -1)):
            P.op('pool', lambda e, m=m: e.memset(m, 1.0), w=['cmask'])
            P.op('pool', lambda e, m=m, pat=pat, cm=cm, base=base: e.affine_select(
                out=m, in_=m, pattern=[[pat, 128]], compare_op=ALU.is_ge, fill=0.0, base=base,
                channel_multiplier=cm), r=['cmask'], w=['cmask'])
        P.op('pool', lambda e: e.memset(self.ones, 1.0), w=['cmask'])
        P.op('pool', lambda e: e.tensor_copy(out=self.m_ge_b, in_=self.m_ge), r=['cmask'], w=['cmask'])
        P.op('pool', lambda e: e.tensor_copy(out=self.ones_b, in_=self.ones), r=['cmask'], w=['cmask'])
        self.CK = ['ident', 'cmask']

    def to_featmajor(self, src, dstT, dkey, ntt=NT, ncols=2048, hook=None):
        P, sb = self.P, self.sb
        mk = sb.mark()
        stg = [sb.alloc([ncols], F32) for _ in range(2)]
        skey = [self.key('fm_stg') for _ in range(2)]
        nk = ncols // 128
        for tt in range(ntt):
            s, sk = stg[tt % 2], skey[tt % 2]
            P.dma(lambda e, s=s, tt=tt: e.dma_start(out=s, in_=src[tt * 128:(tt + 1) * 128, :]), w=[sk])
            for g in range(nk // 4):
                bk, bkey = self.bank()
                for j in range(4):
                    kc = g * 4 + j
                    P.op('pe', lambda e, bk=bk, j=j, s=s, kc=kc: e.transpose(
                        out=bk[:, j * 128:(j + 1) * 128], in_=s[:, kc * 128:(kc + 1) * 128], identity=self.ident),
                        r=[sk, 'ident'], w=[bkey])
                eng = 'dve' if g % 2 == 0 else 'act'
                dst = dstT[:, g * 4:(g + 1) * 4, tt * 128:(tt + 1) * 128]
                srcp = bk.rearrange('p (a b) -> p a b', a=4)
                if eng == 'dve':
                    P.op('dve', lambda e, dst=dst, srcp=srcp: e.tensor_copy(out=dst, in_=srcp), r=[bkey], w=[dkey])
                else:
                    P.op('act', lambda e, dst=dst, srcp=srcp: e.activation(out=dst, in_=srcp, func=AF.Copy), r=[bkey], w=[dkey])
        sb.release(mk)
        P.barrier()

    def wstream_init(self, kc, ncols, nbuf=2):
        sb = self.sb
        self.ws_bf = [sb.alloc([kc, ncols], BF16) for _ in range(nbuf)]
        self.ws_bk = [self.key('ws_bf') for _ in range(nbuf)]
        self.ws_i = 0
        self.ws_n = nbuf

    def wload(self, wap, c0, ncols, kc, q='pool', cast=None):
        P = self.P
        i = self.ws_i % self.ws_n
        self.ws_i += 1
        bf, bk = self.ws_bf[i], self.ws_bk[i]
        src = wap.rearrange('(k p) n -> p k n', p=128)[:, :, c0:c0 + ncols]
        P.dma(lambda e: e.dma_start(out=bf[:, 0:kc, 0:ncols], in_=src), w=[bk], q='pool')
        return bf, bk

    def phase_proj(self, xT, xkey, w_in, proj, ncols):
        P, sb = self.P, self.sb
        mk = sb.mark()
        self.wstream_init(KC, 512, nbuf=3)
        ost = [sb.alloc([512], F32) for _ in range(4)]
        okey = [self.key('ost') for _ in range(4)]
        oi = 0
        nblk = (ncols + 511) // 512
        for nb in range(nblk):
            c0 = nb * 512
            wd = min(512, ncols - c0)
            wb, wk = self.wload(w_in, c0, wd, KC, cast=('pool' if nb % 2 == 0 else 'act'))
            for tt in range(NT):
                bk, bkey = self.bank()
                for kc in range(KC):
                    P.op('pe', lambda e, bk=bk, kc=kc, tt=tt, wb=wb, wd=wd: e.matmul(
                        bk[:, 0:wd], lhsT=xT[:, kc, tt * 128:(tt + 1) * 128], rhs=wb[:, kc, 0:wd],
                        start=(kc == 0), stop=(kc == KC - 1)), r=[xkey, wk], w=[bkey])
                o, ok = ost[oi % 4], okey[oi % 4]
                oi += 1
                if tt % 2 == 0:
                    P.op('dve', lambda e, o=o, bk=bk, wd=wd: e.tensor_copy(out=o[:, 0:wd], in_=bk[:, 0:wd]), r=[bkey], w=[ok])
                else:
                    P.op('act', lambda e, o=o, bk=bk, wd=wd: e.activation(out=o[:, 0:wd], in_=bk[:, 0:wd], func=AF.Copy), r=[bkey], w=[ok])
                P.dma(lambda e, o=o, tt=tt, c0=c0, wd=wd: e.dma_start(
                    out=proj[tt * 128:(tt + 1) * 128, c0:c0 + wd], in_=o[:, 0:wd]), r=[ok], w=['proj'])
        sb.release(mk)
        P.barrier()

    def ln_params(self, ln_g_row, ln_b_row):
        P, sb = self.P, self.sb
        g = sb.alloc([2048], F32)
        b = sb.alloc([2048], F32)
        k = self.key('lnp')
        P.dma(lambda e: e.dma_start(out=g, in_=ln_g_row.partition_broadcast(128)), w=[k])
        P.dma(lambda e: e.dma_start(out=b, in_=ln_b_row.partition_broadcast(128)), w=[k])
        self.ln_small = sb.alloc([4, 8], F32)
        return g, b, k

    def ln_tile(self, acc, akey, g, b, pk, out, okey):
        P = self.P
        sm = self.ln_small
        smk = 'ln_small'
        st = sm[:, :, 0:6]
        for c in range(4):
            P.op('dve', lambda e, c=c: e.bn_stats(out=sm[:, c, 0:6], in_=acc[:, c * 512:(c + 1) * 512]), r=[akey], w=[smk])
        mv = sm[:, 0, 6:8]
        P.op('dve', lambda e: e.bn_aggr(out=mv, in_=st), r=[smk], w=[smk])
        rstd = sm[:, 1, 6:7]
        nmr = sm[:, 1, 7:8]
        P.op('act', lambda e: e.activation(out=rstd, in_=sm[:, 0, 7:8], func=AF.Sqrt, bias=1e-5), r=[smk], w=[smk])
        P.op('dve', lambda e: e.reciprocal(out=rstd, in_=rstd), r=[smk], w=[smk])
        P.op('dve', lambda e: e.scalar_tensor_tensor(out=nmr, in0=sm[:, 0, 6:7], scalar=-1.0, in1=rstd,
                                                      op0=ALU.mult, op1=ALU.mult), r=[smk], w=[smk])
        P.op('act', lambda e: e.activation(out=out, in_=acc, func=AF.Identity, bias=nmr, scale=rstd),
             r=[akey, smk], w=[okey])
        P.op('pool', lambda e: e.tensor_mul(out=out, in0=out, in1=g), r=[okey, pk], w=[okey])
        P.op('dve', lambda e: e.tensor_add(out=out, in0=out, in1=b), r=[okey, pk], w=[okey])

    def end_phase(self, mk):
        self.sb.release(mk)
        self.P.barrier()

    def rms_gate(self, hsrc, hkey, out, okey, g, gk, scal, tmp, tkey, extra=None):
        P = self.P
        ss = tmp[:, 0:1]
        P.op('act', lambda e: e.activation(out=tmp[:, 8:136], in_=hsrc, func=AF.Square, accum_out=ss), r=[hkey], w=[tkey])
        P.op('act', lambda e: e.activation(out=ss, in_=ss, func=AF.Sqrt, bias=1e-6, scale=1.0 / 128), r=[tkey], w=[tkey])
        P.op('dve', lambda e: e.reciprocal(out=ss, in_=ss), r=[tkey], w=[tkey])
        P.op('dve', lambda e: e.tensor_scalar(out=out, in0=hsrc, scalar1=ss, scalar2=float(scal), op0=ALU.mult, op1=ALU.mult),
             r=[hkey, tkey], w=[okey])
        P.op('pool', lambda e: e.tensor_mul(out=out, in0=out, in1=g), r=[okey, gk], w=[okey])

    def phase_da(self, l, proj, ymix, da_lambda, da_norm_g, lambda_init):
        P, sb = self.P, self.sb
        mk = sb.mark()
        cs = sb.alloc([16, 32], F32)
        sn = sb.alloc([16, 32], F32)
        P.dma(lambda e: e.dma_start(out=cs, in_=self.dr['cos'].rearrange('(t p) j -> p t j', p=128)), w=['cs'])
        P.dma(lambda e: e.dma_start(out=sn, in_=self.dr['sin'].rearrange('(t p) j -> p t j', p=128)), w=['cs'])
        gt = sb.alloc([128], F32)
        P.dma(lambda e: e.dma_start(out=gt, in_=da_norm_g.partition_broadcast(128)), w=['da_g'])
        dl = sb.alloc([4, 64], F32)
        P.dma(lambda e: e.dma_start(out=dl, in_=da_lambda.partition_broadcast(128)), w=['dl'])
        lt = sb.alloc([8], F32)
        pr = sb.alloc([2, 64], F32)
        P.op('dve', lambda e: e.tensor_tensor(out=pr[:, 0, :], in0=dl[:, 0, :], in1=dl[:, 1, :], op=ALU.mult), r=['dl'], w=['pr'])
        P.op('dve', lambda e: e.tensor_tensor(out=pr[:, 1, :], in0=dl[:, 2, :], in1=dl[:, 3, :], op=ALU.mult), r=['dl'], w=['pr'])
        P.op('dve', lambda e: e.reduce_sum(out=lt[:, 0:2], in_=pr, axis=AX.X), r=['pr'], w=['lt'])
        P.op('act', lambda e: e.activation(out=lt[:, 2:4], in_=lt[:, 0:2], func=AF.Exp), r=['lt'], w=['lt'])
        P.op('dve', lambda e: e.tensor_tensor(out=lt[:, 4:5], in0=lt[:, 3:4], in1=lt[:, 2:3], op=ALU.subtract), r=['lt'], w=['lt'])
        P.op('dve', lambda e: e.tensor_scalar_add(out=lt[:, 4:5], in0=lt[:, 4:5], scalar1=-float(lambda_init)), r=['lt'], w=['lt'])
        nlam = lt[:, 4:5]
        qk = sb.alloc([16, 256], F32)
        rot = sb.alloc([16, 256], F32)
        t1 = sb.alloc([16, 32], F32)
        t2 = sb.alloc([16, 32], F32)
        qkT = sb.alloc([2, T], BF16)
        vst = sb.alloc([16, 128], F32)
        vx = sb.alloc([16, 132], BF16)
        yst = sb.alloc([16, 128], F32)
        pT = [sb.alloc([16, 128], BF16) for _ in range(4)]
        pk = [self.key('pT') for _ in range(4)]
        tmp = sb.alloc([136], F32)
        osb = sb.alloc([128], F32)
        pi = 0
        P.op('pool', lambda e: e.memset(vx[:, :, 128:129], 1.0), w=['vx1'])
        for h in range(4):
            P.dma(lambda e, h=h: e.dma_start(out=qk[:, :, 0:128], in_=proj[:, C_DAQ + h * 128:C_DAQ + (h + 1) * 128].rearrange('(t p) d -> p t d', p=128)), w=['qk'])
            P.dma(lambda e, h=h: e.dma_start(out=qk[:, :, 128:256], in_=proj[:, C_DAK + h * 128:C_DAK + (h + 1) * 128].rearrange('(t p) d -> p t d', p=128)), w=['qk'])
            P.dma(lambda e, h=h: e.dma_start(out=vst, in_=proj[:, C_DAV + h * 128:C_DAV + (h + 1) * 128].rearrange('(t p) d -> p t d', p=128)), w=['vst'])
            P.op('pool', lambda e: e.tensor_copy(out=vx[:, :, 0:128], in_=vst), r=['vst'], w=['vx'])
            for gI in range(4):
                a = qk[:, :, gI * 64:gI * 64 + 32]
                b = qk[:, :, gI * 64 + 32:gI * 64 + 64]
                o1 = rot[:, :, gI * 64:gI * 64 + 32]
                o2 = rot[:, :, gI * 64 + 32:gI * 64 + 64]
                eng = 'dve' if gI % 2 == 0 else 'pool'
                tk = 'rt%d' % (gI % 2)
                tt_ = t1 if gI % 2 == 0 else t2
                P.op(eng, lambda e, a=a, o1=o1: e.tensor_tensor(out=o1, in0=a, in1=cs, op=ALU.mult), r=['qk', 'cs'], w=['rot'])
                P.op(eng, lambda e, b=b, tt_=tt_: e.tensor_tensor(out=tt_, in0=b, in1=sn, op=ALU.mult), r=['qk', 'cs'], w=[tk])
                P.op(eng, lambda e, o1=o1, tt_=tt_: e.tensor_tensor(out=o1, in0=o1, in1=tt_, op=ALU.subtract), r=['rot', tk], w=['rot'])
                P.op(eng, lambda e, a=a, o2=o2: e.tensor_tensor(out=o2, in0=a, in1=sn, op=ALU.mult), r=['qk', 'cs'], w=['rot'])
                P.op(eng, lambda e, b=b, tt_=tt_: e.tensor_tensor(out=tt_, in0=b, in1=cs, op=ALU.mult), r=['qk', 'cs', 'rot'], w=[tk])
                P.op(eng, lambda e, o2=o2, tt_=tt_: e.tensor_tensor(out=o2, in0=o2, in1=tt_, op=ALU.add), r=['rot', tk], w=['rot'])
            for which in range(2):
                for g4 in range(4):
                    bk, bkey = self.bank()
                    for j in range(4):
                        tt = g4 * 4 + j
                        P.op('pe', lambda e, bk=bk, j=j, tt=tt, which=which: e.transpose(
                            out=bk[:, j * 128:(j + 1) * 128], in_=rot[:, tt, which * 128:(which + 1) * 128], identity=self.ident),
                            r=['rot', 'ident'], w=[bkey])
                    P.op('act', lambda e, bk=bk, g4=g4, which=which: e.activation(
                        out=qkT[:, which, g4 * 512:(g4 + 1) * 512], in_=bk, func=AF.Copy), r=[bkey], w=['qkT'])
            for qi in range(NT):
                obanks = []
                for c in range(2):
                    p_, pkk = pT[pi % 4], pk[pi % 4]
                    pi += 1
                    nkb = qi + 1
                    for g0 in range(0, nkb, 4):
                        n = min(4, nkb - g0)
                        bk, bkey = self.bank()
                        for j in range(n):
                            kb = g0 + j
                            P.op('pe', lambda e, bk=bk, j=j, kb=kb, c=c, qi=qi: e.matmul(
                                bk[:, j * 128:(j + 1) * 128], lhsT=qkT[c * 64:(c + 1) * 64, 1, kb * 128:(kb + 1) * 128],
                                rhs=qkT[c * 64:(c + 1) * 64, 0, qi * 128:(qi + 1) * 128], start=True, stop=True),
                                r=['qkT'], w=[bkey])
                        P.op('act', lambda e, bk=bk, n=n, g0=g0, p_=p_: e.activation(
                            out=p_[:, g0:g0 + n, :], in_=bk[:, 0:n * 128].rearrange('p (a b) -> p a b', a=n),
                            func=AF.Exp, scale=0.125), r=[bkey], w=[pkk])
                    P.op('pool', lambda e, p_=p_, qi=qi: e.tensor_mul(out=p_[:, qi, :], in0=p_[:, qi, :], in1=self.m_ge_b),
                         r=[pkk, 'cmask'], w=[pkk])
                    ob, obkey = self.bank()
                    for kb in range(nkb):
                        P.op('pe', lambda e, ob=ob, kb=kb, p_=p_, nkb=nkb: e.matmul(
                            ob[:, 0:129], lhsT=p_[:, kb, :], rhs=vx[:, kb, 0:129], start=(kb == 0), stop=(kb == nkb - 1)),
                            r=[pkk, 'vx', 'vx1'], w=[obkey])
                    obanks.append((ob, obkey))
                (o0, k0), (o1_, k1) = obanks
                P.op('dve', lambda e, o0=o0: e.reciprocal(out=tmp[:, 1:2], in_=o0[:, 128:129]), r=[k0], w=['datmp'])
                P.op('dve', lambda e, o1_=o1_: e.reciprocal(out=tmp[:, 2:3], in_=o1_[:, 128:129]), r=[k1], w=['datmp'])
                P.op('dve', lambda e: e.tensor_tensor(out=tmp[:, 2:3], in0=tmp[:, 2:3], in1=nlam, op=ALU.mult), r=['datmp', 'lt'], w=['datmp'])
                P.op('dve', lambda e, o0=o0: e.tensor_scalar_mul(out=osb, in0=o0[:, 0:128], scalar1=tmp[:, 1:2]), r=[k0, 'datmp'], w=['osb'])
                P.op('dve', lambda e, o1_=o1_: e.scalar_tensor_tensor(out=osb, in0=o1_[:, 0:128], scalar=tmp[:, 2:3], in1=osb,
                                                                       op0=ALU.mult, op1=ALU.add), r=[k1, 'datmp', 'osb'], w=['osb'])
                self.rms_gate(osb, 'osb', yst[:, qi, :], 'yst', gt, 'da_g', 1.0 - lambda_init, tmp, 'datmp')
                yield
            P.dma(lambda e, h=h: e.dma_start(out=ymix[:, h * 128:(h + 1) * 128].rearrange('(t p) d -> p t d', p=128), in_=yst),
                  r=['yst'], w=[self.key('ymix')])

    def phase_ml(self, l, proj, ymix, ml_gate_b, ml_norm_g):
        P, sb = self.P, self.sb
        mk = sb.mark()
        gt = sb.alloc([128], F32)
        P.dma(lambda e: e.dma_start(out=gt, in_=ml_norm_g.partition_broadcast(128)), w=['ml_g'])
        gb = sb.alloc([2, 4], F32)
        P.dma(lambda e: e.dma_start(out=gb, in_=ml_gate_b.partition_broadcast(128)), w=['gb'])
        gi = sb.alloc([16, 4], F32)
        gf = sb.alloc([16, 4], F32)
        P.dma(lambda e: e.dma_start(out=gi, in_=proj[:, C_MLI:C_MLI + 4].rearrange('(t p) d -> p t d', p=128)), w=['gi'])
        P.dma(lambda e: e.dma_start(out=gf, in_=proj[:, C_MLF:C_MLF + 4].rearrange('(t p) d -> p t d', p=128)), w=['gf'])
        P.op('dve', lambda e: e.tensor_tensor(out=gi, in0=gi, in1=gb[:, 0:1, :].to_broadcast([128, 16, 4]), op=ALU.add), r=['gi', 'gb'], w=['gi'])
        P.op('dve', lambda e: e.tensor_tensor(out=gf, in0=gf, in1=gb[:, 1:2, :].to_broadcast([128, 16, 4]), op=ALU.add), r=['gf', 'gb'], w=['gf'])
        P.op('act', lambda e: e.activation(out=gf, in_=gf, func=AF.Exp, scale=-1.0), r=['gf'], w=['gf'])
        P.op('act', lambda e: e.activation(out=gf, in_=gf, func=AF.Ln, bias=1.0), r=['gf'], w=['gf'])
        P.op('dve', lambda e: e.tensor_scalar_mul(out=gf, in0=gf, scalar1=-1.0), r=['gf'], w=['gf'])
        gf2 = gf.rearrange('p a b -> p (a b)')
        gi2 = gi.rearrange('p a b -> p (a b)')
        aq = sb.alloc([16, 4], F32)
        ak = sb.alloc([16, 4], F32)
        ak2 = sb.alloc([16, 4], F32)
        dec = sb.alloc([16, 4], F32)
        bk, bkey = self.bank()
        P.op('pe', lambda e: e.matmul(bk[:, 0:64], lhsT=self.m_ge, rhs=gf2, start=True, stop=True), r=['gf', 'cmask'], w=[bkey])
        P.op('pe', lambda e: e.matmul(bk[:, 64:128], lhsT=self.ones, rhs=gf2, start=True, stop=True), r=['gf', 'cmask'], w=[bkey])
        aq2, ak_2, ak22, dec2 = (x.rearrange('p a b -> p (a b)') for x in (aq, ak, ak2, dec))
        bsb = sb.alloc([128], F32)
        P.op('act', lambda e: e.activation(out=bsb, in_=bk[:, 0:128], func=AF.Copy), r=[bkey], w=['bsb'])
        P.op('act', lambda e: e.activation(out=aq2, in_=bsb[:, 0:64], func=AF.Exp), r=['bsb'], w=['mlg'])
        P.op('act', lambda e: e.activation(out=dec2, in_=bsb[:, 64:128], func=AF.Exp), r=['bsb'], w=['mlg'])
        P.op('dve', lambda e: e.tensor_tensor(out=ak_2, in0=gi2, in1=bsb[:, 0:64], op=ALU.subtract), r=['gi', 'bsb'], w=['mlg'])
        P.op('dve', lambda e: e.tensor_tensor(out=ak22, in0=ak_2, in1=bsb[:, 64:128], op=ALU.add), r=['bsb', 'mlg'], w=['mlg'])
        P.op('act', lambda e: e.activation(out=ak_2, in_=ak_2, func=AF.Exp), r=['mlg'], w=['mlg'])
        P.op('act', lambda e: e.activation(out=ak22, in_=ak22, func=AF.Exp), r=['mlg'], w=['mlg'])
        qs = sb.alloc([16, 128], F32)
        ks = sb.alloc([16, 128], F32)
        vs = sb.alloc([16, 128], F32)
        osg = sb.alloc([16, 128], F32)
        k2 = sb.alloc([16, 128], BF16)
        vx = sb.alloc([16, 132], BF16)
        qT = sb.alloc([T], BF16)
        kT = sb.alloc([T], BF16)
        yst = sb.alloc([16, 128], F32)
        pTs = [sb.alloc([128], BF16) for _ in range(2)]
        pks = [self.key('mlp') for _ in range(2)]
        M = sb.alloc([132], F32)
        Mb = sb.alloc([132], BF16)
        tmp = sb.alloc([136], F32)
        hsb = sb.alloc([128], F32)
        P.op('pool', lambda e: e.memset(vx[:, :, 128:129], 1.0), w=['mvx1'])
        sc = 128 ** -0.5
        for h in range(4):
            for dst, c0, kk in ((qs, C_MLQ, 'mlq'), (ks, C_MLK, 'mlk'), (vs, C_MLV, 'mlv'), (osg, C_MLO, 'mlo')):
                P.dma(lambda e, dst=dst, c0=c0, h=h: e.dma_start(
                    out=dst, in_=proj[:, c0 + h * 128:c0 + (h + 1) * 128].rearrange('(t p) d -> p t d', p=128)), w=[kk])
            P.op('act', lambda e: e.activation(out=osg, in_=osg, func=AF.Sigmoid), r=['mlo'], w=['mlo'])
            P.op('pool', lambda e: e.tensor_copy(out=vx[:, :, 0:128], in_=vs), r=['mlv'], w=['mvx'])
            P.op('dve', lambda e, h=h: e.tensor_tensor(out=qs, in0=qs, in1=aq[:, :, h:h + 1].to_broadcast([128, 16, 128]), op=ALU.mult), r=['mlq', 'mlg'], w=['mlq'])
            P.op('dve', lambda e, h=h: e.scalar_tensor_tensor(out=k2, in0=ks, scalar=sc, in1=ak2[:, :, h:h + 1].to_broadcast([128, 16, 128]),
                                                               op0=ALU.mult, op1=ALU.mult), r=['mlk', 'mlg'], w=['k2'])
            P.op('dve', lambda e, h=h: e.scalar_tensor_tensor(out=ks, in0=ks, scalar=sc, in1=ak[:, :, h:h + 1].to_broadcast([128, 16, 128]),
                                                                op0=ALU.mult, op1=ALU.mult), r=['mlk', 'mlg', 'k2'], w=['mlk'])
            for src, skey, dstT, dk in ((qs, 'mlq', qT, 'qT'), (ks, 'mlk', kT, 'kT')):
                for g4 in range(4):
                    bk, bkey = self.bank()
                    for j in range(4):
                        tt = g4 * 4 + j
                        P.op('pe', lambda e, bk=bk, j=j, tt=tt, src=src: e.transpose(
                            out=bk[:, j * 128:(j + 1) * 128], in_=src[:, tt, :], identity=self.ident), r=[skey, 'ident'], w=[bkey])
                    P.op('act', lambda e, bk=bk, g4=g4, dstT=dstT: e.activation(out=dstT[:, g4 * 512:(g4 + 1) * 512], in_=bk, func=AF.Copy),
                         r=[bkey], w=[dk])
            for c in range(NT):
                sl = slice(c * 128, (c + 1) * 128)
                p_, pkk = pTs[c % 2], pks[c % 2]
                bk, bkey = self.bank()
                P.op('pe', lambda e, bk=bk, sl=sl: e.matmul(bk[:, 0:128], lhsT=kT[:, sl], rhs=qT[:, sl], start=True, stop=True),
                     r=['qT', 'kT'], w=[bkey])
                P.op('dve', lambda e, bk=bk, p_=p_: e.tensor_tensor(out=p_, in0=bk[:, 0:128], in1=self.m_ge, op=ALU.mult),
                     r=[bkey, 'cmask'], w=[pkk])
                nb, nkey = self.bank()
                P.op('pe', lambda e, nb=nb, p_=p_, c=c: e.matmul(nb[:, 0:129], lhsT=p_, rhs=vx[:, c, 0:129], start=True, stop=(c == 0)),
                     r=[pkk, 'mvx', 'mvx1'], w=[nkey])
                if c > 0:
                    P.op('pe', lambda e, nb=nb, sl=sl: e.matmul(nb[:, 0:129], lhsT=qT[:, sl], rhs=Mb[:, 0:129], start=False, stop=True),
                         r=['qT', 'Mb'], w=[nkey])
                if c < NT - 1:
                    sbk, skey = self.bank()
                    P.op('pe', lambda e, sbk=sbk, c=c: e.matmul(sbk[:, 0:129], lhsT=k2[:, c, :], rhs=vx[:, c, 0:129], start=True, stop=True),
                         r=['k2', 'mvx', 'mvx1'], w=[skey])
                    if c == 0:
                        P.op('dve', lambda e, sbk=sbk: e.tensor_copy(out=M[:, 0:129], in_=sbk[:, 0:129]), r=[skey], w=['M'])
                    else:
                        P.op('dve', lambda e, sbk=sbk, c=c, h=h: e.scalar_tensor_tensor(
                            out=M[:, 0:129], in0=M[:, 0:129], scalar=dec[:, c, h:h + 1], in1=sbk[:, 0:129], op0=ALU.mult, op1=ALU.add),
                            r=[skey, 'M', 'mlg'], w=['M'])
                    P.op('act', lambda e: e.activation(out=Mb[:, 0:129], in_=M[:, 0:129], func=AF.Copy), r=['M'], w=['Mb'])
                P.op('dve', lambda e, nb=nb: e.tensor_copy(out=tmp[:, 3:4], in_=nb[:, 128:129]), r=[nkey], w=['mltmp'])
                P.op('dve', lambda e: e.scalar_tensor_tensor(out=tmp[:, 1:2], in0=tmp[:, 3:4], scalar=-1.0, in1=tmp[:, 3:4],
                                                             op0=ALU.mult, op1=ALU.max), r=['mltmp'], w=['mltmp'])
                P.op('dve', lambda e: e.tensor_scalar_max(out=tmp[:, 1:2], in0=tmp[:, 1:2], scalar1=1.0), r=['mltmp'], w=['mltmp'])
                P.op('dve', lambda e: e.reciprocal(out=tmp[:, 1:2], in_=tmp[:, 1:2]), r=['mltmp'], w=['mltmp'])
                P.op('dve', lambda e, nb=nb: e.tensor_scalar_mul(out=hsb, in0=nb[:, 0:128], scalar1=tmp[:, 1:2]), r=[nkey, 'mltmp'], w=['hsb'])
                self.rms_gate(hsb, 'hsb', yst[:, c, :], 'myst', gt, 'ml_g', 1.0, tmp, 'mltmp')
                yield
            P.op('pool', lambda e: e.tensor_mul(out=yst, in0=yst, in1=osg), r=['myst', 'mlo'], w=['myst'])
            P.dma(lambda e, h=h: e.dma_start(out=ymix[:, 512 + h * 128:512 + (h + 1) * 128].rearrange('(t p) d -> p t d', p=128), in_=yst),
                  r=['myst'], w=[self.key('ymix')])

    def phase_gd(self, l, proj, ymix, conv_w, a_log, dt_bias, gd_norm_g):
        P, sb = self.P, self.sb
        mk = sb.mark()
        TM = lambda c0, h: proj[:, c0 + h * 128:c0 + (h + 1) * 128].rearrange('(t p) d -> p t d', p=128)
        gt = sb.alloc([128], F32)
        P.dma(lambda e: e.dma_start(out=gt, in_=gd_norm_g.partition_broadcast(128)), w=['gd_g'])
        al = sb.alloc([1, 8], F32)
        db = sb.alloc([1, 8], F32)
        P.dma(lambda e: e.dma_start(out=al[:, 0, :], in_=a_log.partition_broadcast(128)), w=['al'])
        P.dma(lambda e: e.dma_start(out=db[:, 0, :], in_=dt_bias.partition_broadcast(128)), w=['db'])
        ga = sb.alloc([16, 8], F32)
        beta = sb.alloc([16, 8], F32)
        t3 = sb.alloc([16, 8], F32)
        P.dma(lambda e: e.dma_start(out=ga, in_=proj[:, C_GDA:C_GDA + 8].rearrange('(t p) d -> p t d', p=128)), w=['ga'])
        P.dma(lambda e: e.dma_start(out=beta, in_=proj[:, C_GDB:C_GDB + 8].rearrange('(t p) d -> p t d', p=128)), w=['beta'])
        P.op('act', lambda e: e.activation(out=beta, in_=beta, func=AF.Sigmoid), r=['beta'], w=['beta'])
        P.op('act', lambda e: e.activation(out=al, in_=al, func=AF.Exp), r=['al'], w=['al'])
        P.op('dve', lambda e: e.tensor_tensor(out=ga, in0=ga, in1=db[:, 0:1, :].to_broadcast([128, 16, 8]), op=ALU.add), r=['ga', 'db'], w=['ga'])
        P.op('dve', lambda e: e.scalar_tensor_tensor(out=t3, in0=ga, scalar=-1.0, in1=ga, op0=ALU.mult, op1=ALU.max), r=['ga'], w=['t3'])
        P.op('act', lambda e: e.activation(out=t3, in_=t3, func=AF.Exp, scale=-1.0), r=['t3'], w=['t3'])
        P.op('act', lambda e: e.activation(out=t3, in_=t3, func=AF.Ln, bias=1.0), r=['t3'], w=['t3'])
        P.op('dve', lambda e: e.tensor_scalar_max(out=ga, in0=ga, scalar1=0.0), r=['ga'], w=['ga'])
        P.op('dve', lambda e: e.tensor_add(out=ga, in0=ga, in1=t3), r=['ga', 't3'], w=['ga'])
        P.op('dve', lambda e: e.scalar_tensor_tensor(out=ga, in0=ga, scalar=-1.0, in1=al[:, 0:1, :].to_broadcast([128, 16, 8]),
                                                      op0=ALU.mult, op1=ALU.mult), r=['ga', 'al'], w=['ga'])
        ga2 = ga.rearrange('p a b -> p (a b)')
        egc = sb.alloc([16, 8], F32)
        ekg = sb.alloc([16, 8], F32)
        egl = sb.alloc([16, 8], F32)
        bk, bkey = self.bank()
        P.op('pe', lambda e: e.matmul(bk[:, 0:128], lhsT=self.m_ge, rhs=ga2, start=True, stop=True), r=['ga', 'cmask'], w=[bkey])
        P.op('pe', lambda e: e.matmul(bk[:, 128:256], lhsT=self.ones, rhs=ga2, start=True, stop=True), r=['ga', 'cmask'], w=[bkey])
        f2 = lambda x: x.rearrange('p a b -> p (a b)')
        P.op('act', lambda e: e.activation(out=f2(egc), in_=bk[:, 0:128], func=AF.Exp), r=[bkey], w=['gdg'])
        P.op('act', lambda e: e.activation(out=f2(egl), in_=bk[:, 128:256], func=AF.Exp), r=[bkey], w=['gdg'])
        P.op('act', lambda e: e.activation(out=f2(t3), in_=bk[:, 0:128], func=AF.Copy), r=[bkey, 't3'], w=['t3'])
        P.op('dve', lambda e: e.tensor_tensor(out=f2(ekg), in0=bk[:, 128:256], in1=f2(t3), op=ALU.subtract), r=[bkey, 't3'], w=['ekg'])
        P.op('act', lambda e: e.activation(out=f2(ekg), in_=f2(ekg), func=AF.Exp), r=['ekg'], w=['ekg'])
        import os
        GS = os.environ.get('GD_STOP', '')
        if GS == 'gates':
            self.end_phase(mk)
            return
        xs = [sb.alloc([16, 128], F32) for _ in range(4)]
        cw = sb.alloc([4, 3, 128], F32)
        cv = [sb.alloc([16, 128], F32) for _ in range(3)]
        ckey = ['gq', 'gk', 'gv']
        sq = sb.alloc([16, 128], F32)
        ssq = sb.alloc([16, 1], F32)
        kbeta = sb.alloc([16, 128], F32)
        vb = sb.alloc([16, 128], F32)
        kbg = sb.alloc([16, 128], F32)
        qg = sb.alloc([16, 128], F32)
        BS = [(sb.alloc([16, 128], F32), sb.alloc([16, 128], BF16), sb.alloc([16, 128], BF16), sb.alloc([16, 128], BF16), sb.alloc([16, 128], BF16)) for _ in range(2)]
        zt = sb.alloc([16, 128], F32)
        G = 4
        TR = sb.alloc([G, 4, 128], F32)
        kTt = TR[:, :, 0, :]
        kbTt = TR[:, :, 1, :]
        qTt = TR[:, :, 2, :]
        Et = sb.alloc([G, 128], F32)
        utg = sb.alloc([G, 128], F32)
        Bt = [sb.alloc([G, 128], F32) for _ in range(2)]
        Nt = [sb.alloc([G, 128], F32) for _ in range(2)]
        Rt = [sb.alloc([G, 128], F32) for _ in range(2)]
        S = sb.alloc([128], F32)
        Sb = sb.alloc([128], BF16)
        vn = sb.alloc([128], BF16)
        yst = sb.alloc([16, 128], F32)
        tmp = sb.alloc([136], F32)
        osb = sb.alloc([128], F32)
        for x_ in xs[0:3]:
            P.op('pool', lambda e, x_=x_: e.memset(x_[0:32, 0, :], 0.0), w=['xs'])
        sc = 128 ** -0.5
        def front(h, sidx):
            U, WT, AT, QGT, kgs = BS[sidx]
            kU, kWT, kAT, kQGT, kkgs = ('%s%d' % (n_, sidx) for n_ in ('U', 'WT', 'AT', 'QGT', 'kgs'))
            for j in range(3):
                P.dma(lambda e, h=h, j=j: e.dma_start(
                    out=cw[:, :, j, :], in_=conv_w[:, j * 1024 + h * 128:j * 1024 + (h + 1) * 128].partition_broadcast(128)), w=['cw'])
            for j in range(3):
                src = TM(C_GDQKV + j * 1024, h)
                xk = 'xs%d' % j
                for i in range(4):
                    s_ = 3 - i
                    if s_ == 0:
                        P.dma(lambda e, src=src: e.dma_start(out=xs[3], in_=src), r=['xs'], w=[xk])
                    else:
                        P.dma(lambda e, src=src, i=i, s_=s_: e.dma_start(out=xs[i][s_:128, :, :], in_=src[0:128 - s_, :, :]), r=['xs'], w=[xk])
                        P.dma(lambda e, src=src, i=i, s_=s_: e.dma_start(out=xs[i][0:s_, 1:16, :], in_=src[128 - s_:128, 0:15, :]), r=['xs'], w=[xk])
                acc = cv[j]
                eng = 'dve' if j != 1 else 'pool'
                wvs = [cw[:, i:i + 1, j, :].to_broadcast([128, 16, 128]) for i in range(4)]
                P.op(eng, lambda e, acc=acc, w3=wvs[3]: e.tensor_tensor(out=acc, in0=xs[3], in1=w3, op=ALU.mult), r=[xk, 'cw'], w=[ckey[j]])
                for i in range(3):
                    P.op(eng, lambda e, i=i, wi_=wvs[i]: e.tensor_tensor(out=xs[i], in0=xs[i], in1=wi_, op=ALU.mult), r=[xk, 'cw'], w=[xk])
                    P.op(eng, lambda e, i=i, acc=acc: e.tensor_add(out=acc, in0=acc, in1=xs[i]), r=[xk, ckey[j]], w=[ckey[j]])
                P.op('act', lambda e, acc=acc: e.activation(out=acc, in_=acc, func=AF.Silu), r=[ckey[j]], w=[ckey[j]])
                P.op('pool', lambda e: e.memset(xs[0][0:32, 0, :], 0.0), w=['xs', xk])
                P.op('pool', lambda e: e.memset(xs[1][0:32, 0, :], 0.0), w=['xs', xk])
                P.op('pool', lambda e: e.memset(xs[2][0:32, 0, :], 0.0), w=['xs', xk])
                if j < 2:
                    P.op('pool', lambda e, acc=acc: e.tensor_mul(out=sq, in0=acc, in1=acc), r=[ckey[j]], w=['sq'])
                    P.op('dve', lambda e: e.reduce_sum(out=ssq[:, :, 0], in_=sq, axis=AX.X), r=['sq'], w=['ssq'])
                    P.op('act', lambda e: e.activation(out=ssq, in_=ssq, func=AF.Sqrt, bias=1e-6), r=['ssq'], w=['ssq'])
                    P.op('dve', lambda e: e.reciprocal(out=ssq, in_=ssq), r=['ssq'], w=['ssq'])
                    P.op('dve', lambda e, acc=acc, j=j: e.scalar_tensor_tensor(
                        out=acc, in0=acc, scalar=(sc if j == 0 else 1.0), in1=ssq.to_broadcast([128, 16, 128]),
                        op0=ALU.mult, op1=ALU.mult), r=[ckey[j], 'ssq'], w=[ckey[j]])
                yield
            qn, kn, vv = cv
            bc = lambda t_, h=h: t_[:, :, h:h + 1].to_broadcast([128, 16, 128])
            b_beta, b_egc, b_ekg = bc(beta), bc(egc), bc(ekg)
            P.op('dve', lambda e, b_beta=b_beta: e.tensor_tensor(out=kbeta, in0=kn, in1=b_beta, op=ALU.mult), r=['gk', 'beta'], w=['kbeta'])
            P.op('pool', lambda e, b_beta=b_beta: e.tensor_tensor(out=vb, in0=vv, in1=b_beta, op=ALU.mult), r=['gv', 'beta'], w=['vb'])
            P.op('dve', lambda e, b_egc=b_egc: e.tensor_tensor(out=kbg, in0=kbeta, in1=b_egc, op=ALU.mult), r=['kbeta', 'gdg'], w=['kbg'])
            P.op('pool', lambda e, b_egc=b_egc: e.tensor_tensor(out=qg, in0=qn, in1=b_egc, op=ALU.mult), r=['gq', 'gdg'], w=['qg'])
            P.op('dve', lambda e, b_ekg=b_ekg: e.tensor_tensor(out=kgs, in0=kn, in1=b_ekg, op=ALU.mult), r=['gk', 'ekg'], w=[kkgs])
            yield
            for c0 in range(0, NT, G):
                tiles = list(range(c0, c0 + G))
                gk_ = lambda s, i: '%s_%d' % (s, i)
                for i, c in enumerate(tiles):
                    bk, bkey = self.bank()
                    for j, (src, skey) in enumerate(((kn, 'gk'), (kbeta, 'kbeta'), (qn, 'gq'), (qg, 'qg'))):
                        P.op('pe', lambda e, bk=bk, j=j, src=src, c=c: e.transpose(out=bk[:, j * 128:(j + 1) * 128], in_=src[:, c, :], identity=self.ident),
                             r=[skey, 'ident'], w=[bkey])
                    ev = 'act' if i % 2 == 0 else 'dve'
                    if ev == 'act':
                        P.op('act', lambda e, bk=bk, i=i: e.activation(out=TR[:, i, :, :], in_=bk.rearrange('p (a b) -> p a b', a=4), func=AF.Copy),
                             r=[bkey], w=[gk_('kT', i), gk_('kbT', i), gk_('qT', i), gk_('qgT', i)])
                    else:
                        P.op('dve', lambda e, bk=bk, i=i: e.tensor_copy(out=TR[:, i, :, :], in_=bk.rearrange('p (a b) -> p a b', a=4)),
                             r=[bkey], w=[gk_('kT', i), gk_('kbT', i), gk_('qT', i), gk_('qgT', i)])
                    P.op('pool', lambda e, i=i, c=c: e.tensor_copy(out=QGT[:, c, :], in_=TR[:, i, 3, :]), r=[gk_('qgT', i)], w=[kQGT])
                    GV = ''
                    if GV != 'noutg':
                        P.op('dve', lambda e, i=i, c=c, h=h: e.tensor_scalar_mul(out=utg[:, i, :], in0=self.m_ge, scalar1=ga[:, c, h:h + 1]),
                             r=['cmask', 'ga'], w=[gk_('utg', i)])
                yield
                banks = []
                for i, c in enumerate(tiles):
                    bk, bkey = self.bank()
                    banks.append((bk, bkey))
                    P.op('pe', lambda e, bk=bk, i=i: e.matmul(bk[:, 0:128], lhsT=self.m_lt, rhs=utg[:, i, :], start=True, stop=True),
                         r=['cmask', gk_('utg', i)], w=[bkey])
                    P.op('pe', lambda e, bk=bk, i=i: e.matmul(bk[:, 128:256], lhsT=kTt[:, i, :], rhs=kbTt[:, i, :], start=True, stop=True),
                         r=[gk_('kT', i), gk_('kbT', i)], w=[bkey])
                    P.op('pe', lambda e, bk=bk, i=i: e.matmul(bk[:, 256:384], lhsT=kTt[:, i, :], rhs=qTt[:, i, :], start=True, stop=True),
                         r=[gk_('kT', i), gk_('qT', i)], w=[bkey])
                for i, c in enumerate(tiles):
                    bk, bkey = banks[i]
                    P.op('act', lambda e, bk=bk, i=i: e.activation(out=Et[:, i, :], in_=bk[:, 0:128], func=AF.Exp), r=[bkey], w=[gk_('E', i)])
                    P.op('dve', lambda e, bk=bk, i=i: e.scalar_tensor_tensor(out=Bt[0][:, i, :], in0=bk[:, 128:256], scalar=-1.0, in1=Et[:, i, :],
                                                                              op0=ALU.mult, op1=ALU.mult), r=[bkey, gk_('E', i)], w=[gk_('B0', i)])
                    P.op('pool', lambda e, i=i: e.tensor_mul(out=Bt[0][:, i, :], in0=Bt[0][:, i, :], in1=self.m_gt), r=[gk_('B0', i), 'cmask'], w=[gk_('B0', i)])
                    P.op('dve', lambda e, bk=bk, i=i: e.tensor_tensor(out=Et[:, i, :], in0=bk[:, 256:384], in1=Et[:, i, :], op=ALU.mult),
                         r=[bkey, gk_('E', i)], w=[gk_('E', i)])
                    P.op('pool', lambda e, i=i, c=c: e.tensor_mul(out=AT[:, c, :], in0=Et[:, i, :], in1=self.m_ge), r=[gk_('E', i), 'cmask'], w=[kAT])
                yield
                banks = []
                for i, c in enumerate(tiles):
                    bk, bkey = self.bank()
                    banks.append((bk, bkey))
                    P.op('pe', lambda e, bk=bk, i=i: e.transpose(out=bk[:, 0:128], in_=Bt[0][:, i, :], identity=self.ident),
                         r=[gk_('B0', i), 'ident'], w=[bkey])
                for i, c in enumerate(tiles):
                    bk, bkey = banks[i]
                    P.op('act', lambda e, bk=bk, i=i: e.activation(out=Nt[0][:, i, :], in_=bk[:, 0:128], func=AF.Copy), r=[bkey], w=[gk_('N0', i)])
                    P.op('dve', lambda e, i=i: e.tensor_add(out=Rt[0][:, i, :], in0=Bt[0][:, i, :], in1=self.ident), r=[gk_('B0', i), 'ident'], w=[gk_('R0', i)])
                yield
                NLEV = 6
                for lev in range(NLEV):
                    a, b = lev % 2, (lev + 1) % 2
                    last = (lev == NLEV - 1)
                    yield
                    banks = []
                    for i, c in enumerate(tiles):
                        bk, bkey = self.bank()
                        banks.append((bk, bkey))
                        P.op('pe', lambda e, bk=bk, i=i, a=a: e.matmul(bk[:, 0:128], lhsT=Bt[a][:, i, :], rhs=Nt[a][:, i, :], start=True, stop=True),
                             r=[gk_('B%d' % a, i), gk_('N%d' % a, i)], w=[bkey])
                        if not last:
                            P.op('pe', lambda e, bk=bk, i=i, a=a: e.matmul(bk[:, 128:256], lhsT=Nt[a][:, i, :], rhs=Bt[a][:, i, :], start=True, stop=True),
                                 r=[gk_('B%d' % a, i), gk_('N%d' % a, i)], w=[bkey])
                    yield
                    for i, c in enumerate(tiles):
                        bk, bkey = banks[i]
                        if i % 2 == 0:
                            P.op('act', lambda e, bk=bk, i=i, b=b: e.activation(out=Nt[b][:, i, :], in_=bk[:, 0:128], func=AF.Copy), r=[bkey], w=[gk_('N%d' % b, i)])
                            if not last:
                                P.op('act', lambda e, bk=bk, i=i, b=b: e.activation(out=Bt[b][:, i, :], in_=bk[:, 128:256], func=AF.Copy), r=[bkey], w=[gk_('B%d' % b, i)])
                        else:
                            P.op('dve', lambda e, bk=bk, i=i, b=b: e.tensor_copy(out=Nt[b][:, i, :], in_=bk[:, 0:128]), r=[bkey], w=[gk_('N%d' % b, i)])
                            if not last:
                                P.op('dve', lambda e, bk=bk, i=i, b=b: e.tensor_copy(out=Bt[b][:, i, :], in_=bk[:, 128:256]), r=[bkey], w=[gk_('B%d' % b, i)])
                    yield
                    for i, c in enumerate(tiles):
                        bk, bkey = banks[i]
                        P.op('pe', lambda e, bk=bk, i=i, a=a, b=b: e.matmul(bk[:, 256:384], lhsT=Nt[b][:, i, :], rhs=Rt[a][:, i, :], start=True, stop=True),
                             r=[gk_('N%d' % b, i), gk_('R%d' % a, i)], w=[bkey])
                    yield
                    for i, c in enumerate(tiles):
                        bk, bkey = banks[i]
                        P.op('dve', lambda e, bk=bk, i=i, a=a, b=b: e.tensor_tensor(out=Rt[b][:, i, :], in0=bk[:, 256:384], in1=Rt[a][:, i, :], op=ALU.add),
                             r=[bkey, gk_('R%d' % a, i)], w=[gk_('R%d' % b, i)])
                yield
                rf = NLEV % 2
                for i, c in enumerate(tiles):
                    bk, bkey = self.bank()
                    P.op('pe', lambda e, bk=bk, i=i, c=c: e.matmul(bk[:, 0:128], lhsT=Rt[rf][:, i, :], rhs=vb[:, c, :], start=True, stop=True),
                         r=[gk_('R%d' % rf, i), 'vb'], w=[bkey])
                    P.op('pe', lambda e, bk=bk, i=i, c=c: e.matmul(bk[:, 128:256], lhsT=kbg[:, c, :], rhs=Rt[rf][:, i, :], start=True, stop=True),
                         r=[gk_('R%d' % rf, i), 'kbg'], w=[bkey])
                    P.op('dve', lambda e, bk=bk, c=c: e.tensor_copy(out=U[:, c, :], in_=bk[:, 0:128]), r=[bkey], w=[kU])
                    P.op('dve', lambda e, bk=bk, c=c: e.tensor_copy(out=WT[:, c, :], in_=bk[:, 128:256]), r=[bkey], w=[kWT])
        def back(h, sidx):
            U, WT, AT, QGT, kgs = BS[sidx]
            kU, kWT, kAT, kQGT, kkgs = ('%s%d' % (n_, sidx) for n_ in ('U', 'WT', 'AT', 'QGT', 'kgs'))
            P.dma(lambda e, h=h: e.dma_start(out=zt, in_=TM(C_GDZ, h)), w=['zt'])
            P.op('act', lambda e: e.activation(out=zt, in_=zt, func=AF.Silu), r=['zt'], w=['zt'])
            for c in range(NT):
                if c == 0:
                    P.op('dve', lambda e: e.tensor_copy(out=vn, in_=U[:, 0, :]), r=[kU], w=['vn'])
                else:
                    bk, bkey = self.bank()
                    P.op('pe', lambda e, bk=bk, c=c: e.matmul(bk[:, 0:128], lhsT=WT[:, c, :], rhs=Sb, start=True, stop=True), r=[kWT, 'Sb'], w=[bkey])
                    P.op('dve', lambda e, bk=bk, c=c: e.tensor_tensor(out=vn, in0=U[:, c, :], in1=bk[:, 0:128], op=ALU.subtract), r=[kU, bkey], w=['vn'])
                ob, okey = self.bank()
                if c > 0:
                    P.op('pe', lambda e, ob=ob, c=c: e.matmul(ob[:, 0:128], lhsT=QGT[:, c, :], rhs=Sb, start=True, stop=False), r=[kQGT, 'Sb'], w=[okey])
                P.op('pe', lambda e, ob=ob, c=c: e.matmul(ob[:, 0:128], lhsT=AT[:, c, :], rhs=vn, start=(c == 0), stop=True), r=[kAT, 'vn'], w=[okey])
                if c < NT - 1:
                    db_, dkey = self.bank()
                    P.op('pe', lambda e, db_=db_, c=c: e.matmul(db_[:, 0:128], lhsT=kgs[:, c, :], rhs=vn, start=True, stop=True), r=[kkgs, 'vn'], w=[dkey])
                    if c == 0:
                        P.op('dve', lambda e, db_=db_: e.tensor_copy(out=S, in_=db_[:, 0:128]), r=[dkey], w=['S'])
                    else:
                        P.op('dve', lambda e, db_=db_, c=c, h=h: e.scalar_tensor_tensor(out=S, in0=S, scalar=egl[:, c, h:h + 1], in1=db_[:, 0:128],
                                                                                      op0=ALU.mult, op1=ALU.add), r=[dkey, 'S', 'gdg'], w=['S'])
                    P.op('act', lambda e: e.activation(out=Sb, in_=S, func=AF.Copy), r=['S'], w=['Sb'])
                P.op('act', lambda e, ob=ob: e.activation(out=osb, in_=ob[:, 0:128], func=AF.Copy), r=[okey], w=['osb'])
                self.rms_gate(osb, 'osb', yst[:, c, :], 'yst', gt, 'gd_g', 1.0, tmp, 'gdtmp')
                yield
            P.op('pool', lambda e: e.tensor_mul(out=yst, in0=yst, in1=zt), r=['yst', 'zt'], w=['yst'])
            P.dma(lambda e, h=h: e.dma_start(out=ymix[:, 1024 + h * 128:1024 + (h + 1) * 128].rearrange('(t p) d -> p t d', p=128), in_=yst),
                  r=['yst'], w=[self.key('ymix')])

        def step(g, n):
            for _ in range(n):
                try:
                    next(g)
                except StopIteration:
                    return True
            return False

        g0 = front(0, 0)
        while not step(g0, 1000):
            pass
        for h in range(8):
            gb = back(h, h % 2)
            gf = front(h + 1, (h + 1) % 2) if h < 7 else None
            done_b = False
            done_f = gf is None
            while not (done_b and done_f):
                if not done_b:
                    done_b = step(gb, 1)
                if not done_f:
                    done_f = step(gf, 4)
        self.end_phase(mk)

    def proj_feat(self, xT, xkey, W, outT, okey, ntok=T, ncols=2048):
        P = self.P
        for nb in range(ncols // 128):
            wb, wk = self.wload(W, nb * 128, 128, KC, cast=('pool' if nb % 2 == 0 else 'dve'))
            for tb in range(max(1, ntok // 512)):
                n = min(512, ntok)
                bk, bkey = self.bank()
                for kc in range(KC):
                    P.op('pe', lambda e, bk=bk, kc=kc, wb=wb, tb=tb, n=n: e.matmul(
                        bk[:, 0:n], lhsT=wb[:, kc, 0:128], rhs=xT[:, kc, tb * 512:tb * 512 + n],
                        start=(kc == 0), stop=(kc == KC - 1)), r=[xkey, wk], w=[bkey])
                P.op('act', lambda e, bk=bk, nb=nb, tb=tb, n=n: e.activation(out=outT[:, nb, tb * 512:tb * 512 + n], in_=bk[:, 0:n], func=AF.Copy),
                     r=[bkey], w=[okey])

    def phase_xa(self, l, xT, xkey, mem, wq, wkv, qT):
        P, sb = self.P, self.sb
        memT = sb.alloc([KC, 256], BF16)
        self.to_featmajor(mem, memT, 'memT', ntt=2)
        self.wstream_init(KC, 128, nbuf=3)
        kT = sb.alloc([16, 256], BF16)
        V = sb.alloc([2, 2048], BF16)
        self.proj_feat(memT, 'memT', wkv, kT, 'xkT', ntok=256, ncols=2048)
        wv = wkv[:, 2048:4096]
        for nb in range(16):
            wb, wk = self.wload(wv, nb * 128, 128, KC, cast=('pool' if nb % 2 == 0 else 'dve'))
            for mt in range(2):
                bk, bkey = self.bank()
                for kc in range(KC):
                    P.op('pe', lambda e, bk=bk, kc=kc, wb=wb, mt=mt: e.matmul(
                        bk[:, 0:128], lhsT=memT[:, kc, mt * 128:(mt + 1) * 128], rhs=wb[:, kc, 0:128], start=(kc == 0), stop=(kc == KC - 1)),
                        r=['memT', wk], w=[bkey])
                P.op('act', lambda e, bk=bk, mt=mt, nb=nb: e.activation(out=V[:, mt, nb * 128:(nb + 1) * 128], in_=bk[:, 0:128], func=AF.Copy),
                     r=[bkey], w=['xV'])
        self.proj_feat(xT, xkey, wq, qT, 'xqT')
        pT = [sb.alloc([2, 512], BF16) for _ in range(2)]
        pk = [self.key('xp') for _ in range(2)]
        rs = sb.alloc([512], F32)
        sc = 512 ** -0.5
        it = 0
        for hd in range(4):
            for tb in range(4):
                p_, pkk = pT[it % 2], pk[it % 2]
                it += 1
                tsl = slice(tb * 512, (tb + 1) * 512)
                for mt in range(2):
                    bk, bkey = self.bank()
                    for dc in range(4):
                        P.op('pe', lambda e, bk=bk, dc=dc, mt=mt, hd=hd, tsl=tsl: e.matmul(
                            bk, lhsT=kT[:, hd * 4 + dc, mt * 128:(mt + 1) * 128], rhs=qT[:, hd * 4 + dc, tsl], start=(dc == 0), stop=(dc == 3)),
                            r=['xkT', 'xqT'], w=[bkey])
                    P.op('act', lambda e, bk=bk, mt=mt, p_=p_: e.activation(out=p_[:, mt, :], in_=bk, func=AF.Exp, scale=sc), r=[bkey], w=[pkk])
                sbk, skey = self.bank()
                for mt in range(2):
                    P.op('pe', lambda e, sbk=sbk, mt=mt, p_=p_: e.matmul(sbk, lhsT=self.ones_b, rhs=p_[:, mt, :], start=(mt == 0), stop=(mt == 1)),
                         r=[pkk, 'cmask'], w=[skey])
                P.op('dve', lambda e, sbk=sbk: e.reciprocal(out=rs, in_=sbk), r=[skey], w=['xrs'])
                for dvc in range(4):
                    bk, bkey = self.bank()
                    for mt in range(2):
                        P.op('pe', lambda e, bk=bk, mt=mt, p_=p_, dvc=dvc, hd=hd: e.matmul(
                            bk, lhsT=V[:, mt, hd * 512 + dvc * 128:hd * 512 + (dvc + 1) * 128], rhs=p_[:, mt, :], start=(mt == 0), stop=(mt == 1)),
                            r=[pkk, 'xV'], w=[bkey])
                    P.op('dve', lambda e, bk=bk, dvc=dvc, hd=hd, tsl=tsl: e.tensor_tensor(out=qT[:, hd * 4 + dvc, tsl], in0=bk, in1=rs, op=ALU.mult),
                         r=[bkey, 'xrs', 'xqT'], w=['xqT'])

    def route_tile(self, tt, hs, hkey, rw, rbias, comb, sm):
        P = self.P
        hT = self.rt_hT
        mxT = self.moe_xT
        for g in range(4):
            bk, bkey = self.bank()
            for j in range(4):
                kc = g * 4 + j
                P.op('pe', lambda e, bk=bk, j=j, kc=kc: e.transpose(out=bk[:, j * 128:(j + 1) * 128], in_=hs[:, kc * 128:(kc + 1) * 128], identity=self.ident),
                     r=[hkey, 'ident'], w=[bkey])
            P.op('act', lambda e, bk=bk, g=g: e.activation(out=hT[:, g * 4:(g + 1) * 4, :], in_=bk.rearrange('p (a b) -> p a b', a=4), func=AF.Copy),
                 r=[bkey], w=['rt_hT'])
            P.op('pool', lambda e, g=g, tt=tt: e.tensor_copy(out=mxT[:, g * 4:(g + 1) * 4, tt * 128:(tt + 1) * 128],
                                                             in_=hT[:, g * 4:(g + 1) * 4, :]), r=['rt_hT'], w=['moe_xT'])
        bk, bkey = self.bank()
        for kc in range(KC):
            P.op('pe', lambda e, bk=bk, kc=kc: e.matmul(bk[:, 0:16], lhsT=hT[:, kc, :], rhs=rw[:, kc, :], start=(kc == 0), stop=(kc == KC - 1)),
                 r=['rt_hT', 'rw'], w=[bkey])
        k = 'rsm'
        s, sel, t1, t2 = sm[:, 0, :], sm[:, 1, :], sm[:, 2, :], sm[:, 3, :]
        m4, m4b, gm = sm[:, 4, 0:4], sm[:, 4, 4:8], sm[:, 4, 8:9]
        e1 = sm[:, 4, 9:10]
        BIG = 1.0e4
        v3 = lambda a: a.rearrange('p (g s) -> p g s', g=4)
        P.op('act', lambda e, bk=bk: e.activation(out=s, in_=bk[:, 0:16], func=AF.Sigmoid), r=[bkey], w=[k])
        P.op('dve', lambda e: e.tensor_add(out=sel, in0=s, in1=rbias), r=[k, 'rbias'], w=[k])
        P.op('dve', lambda e: e.reduce_max(out=m4, in_=v3(sel), axis=AX.X), r=[k], w=[k])
        P.op('dve', lambda e: e.tensor_tensor(out=v3(t1), in0=v3(sel), in1=m4.rearrange('p (g o) -> p g o', o=1).to_broadcast([128, 4, 4]), op=ALU.is_equal), r=[k], w=[k])
        P.op('dve', lambda e: e.scalar_tensor_tensor(out=t1, in0=t1, scalar=-BIG, in1=sel, op0=ALU.mult, op1=ALU.add), r=[k], w=[k])
        P.op('dve', lambda e: e.reduce_max(out=m4b, in_=v3(t1), axis=AX.X), r=[k], w=[k])
        P.op('dve', lambda e: e.tensor_add(out=m4, in0=m4, in1=m4b), r=[k], w=[k])
        P.op('dve', lambda e: e.reduce_max(out=gm, in_=m4, axis=AX.X), r=[k], w=[k])
        P.op('dve', lambda e: e.tensor_scalar(out=m4b, in0=m4, scalar1=gm, scalar2=None, op0=ALU.is_equal), r=[k], w=[k])
        P.op('dve', lambda e: e.tensor_scalar(out=m4b, in0=m4b, scalar1=-1.0, scalar2=BIG, op0=ALU.add, op1=ALU.mult), r=[k], w=[k])
        P.op('dve', lambda e: e.tensor_tensor(out=v3(t1), in0=v3(sel), in1=m4b.rearrange('p (g o) -> p g o', o=1).to_broadcast([128, 4, 4]), op=ALU.add), r=[k], w=[k])
        P.op('dve', lambda e: e.reduce_max(out=e1, in_=t1, axis=AX.X), r=[k], w=[k])
        P.op('dve', lambda e: e.tensor_scalar(out=t2, in0=t1, scalar1=e1, scalar2=None, op0=ALU.is_equal), r=[k], w=[k])
        P.op('dve', lambda e: e.scalar_tensor_tensor(out=t1, in0=t2, scalar=-BIG, in1=t1, op0=ALU.mult, op1=ALU.add), r=[k], w=[k])
        P.op('dve', lambda e: e.reduce_max(out=e1, in_=t1, axis=AX.X), r=[k], w=[k])
        P.op('dve', lambda e: e.tensor_scalar(out=t1, in0=t1, scalar1=e1, scalar2=None, op0=ALU.is_equal), r=[k], w=[k])
        P.op('dve', lambda e: e.tensor_add(out=t1, in0=t1, in1=t2), r=[k], w=[k])
        P.op('dve', lambda e: e.tensor_mul(out=t1, in0=t1, in1=s), r=[k], w=[k])
        P.op('dve', lambda e: e.reduce_sum(out=e1, in_=t1, axis=AX.X), r=[k], w=[k])
        P.op('dve', lambda e: e.reciprocal(out=e1, in_=e1), r=[k], w=[k])
        P.op('dve', lambda e, tt=tt: e.tensor_scalar_mul(out=comb[:, tt, :], in0=t1, scalar1=e1), r=[k], w=['comb'])

    def phase_moe(self, l, hsrc, hdst, router_w, router_bias, w_in, w_out, ln_g_row, ln_b_row):
        P, sb = self.P, self.sb
        mk = sb.mark()
        comb = sb.alloc([16, 16], F32)
        rw = sb.alloc([KC, 16], F32)
        rbias = sb.alloc([16], F32)
        sm = sb.alloc([5, 16], F32)
        P.dma(lambda e: e.dma_start(out=rw, in_=router_w.rearrange('(k p) n -> p k n', p=128)), w=['rw'])
        P.dma(lambda e: e.dma_start(out=rbias, in_=router_bias.partition_broadcast(128)), w=['rbias'])
        g, b, pk = self.ln_params(ln_g_row, ln_b_row)
        xT = sb.alloc([KC, 1024], BF16)
        self.moe_xT = xT
        acc = sb.alloc([8, 2048], F32)
        actT = sb.alloc([8, 1024], BF16)
        sl = sb.alloc([512], F32)
        hs2 = [sb.alloc([2048], F32) for _ in range(2)]
        hk2 = [self.key('mhs') for _ in range(2)]
        mk2 = sb.mark()
        for hf in range(2):
            self.rt_hT = sb.alloc([KC, 128], F32)
            for t8 in range(8):
                tt = hf * 8 + t8
                hs, hkk = hs2[t8 % 2], hk2[t8 % 2]
                P.dma(lambda e, hs=hs, tt=tt: e.dma_start(out=hs, in_=hsrc[tt * 128:(tt + 1) * 128, :]), w=[hkk])
                self.route_tile(t8, hs, hkk, rw, rbias, comb[:, hf * 8:(hf + 1) * 8, :], sm)
            sb.release(mk2)
            P.barrier()
            self.wstream_init(KC, 256, nbuf=4)
            wo_bf = [sb.alloc([8, 256], BF16) for _ in range(3)]
            wo_bk = [self.key('wo_b') for _ in range(3)]
            woi = 0
            for ex in range(16):
                wi = w_in[ex]
                for fcp in range(4):
                    wg, wgk = self.wload(wi, fcp * 256, 256, KC)
                    wu, wuk = self.wload(wi, 1024 + fcp * 256, 256, KC)
                    for j2 in range(2):
                        fc = fcp * 2 + j2
                        for tb in range(2):
                            tsl = slice(tb * 512, (tb + 1) * 512)
                            gb_, gkey = self.bank()
                            ub_, ukey = self.bank()
                            for kc in range(KC):
                                P.op('pe', lambda e, gb_=gb_, kc=kc, wg=wg, tsl=tsl, j2=j2: e.matmul(gb_, lhsT=wg[:, kc, j2 * 128:(j2 + 1) * 128], rhs=xT[:, kc, tsl],
                                                                                                  start=(kc == 0), stop=(kc == KC - 1)), r=['moe_xT', wgk], w=[gkey])
                            for kc in range(KC):
                                P.op('pe', lambda e, ub_=ub_, kc=kc, wu=wu, tsl=tsl, j2=j2: e.matmul(ub_, lhsT=wu[:, kc, j2 * 128:(j2 + 1) * 128], rhs=xT[:, kc, tsl],
                                                                                                  start=(kc == 0), stop=(kc == KC - 1)), r=['moe_xT', wuk], w=[ukey])
                            P.op('act', lambda e, gb_=gb_: e.activation(out=sl, in_=gb_, func=AF.Silu), r=[gkey], w=['sl'])
                            P.op('dve', lambda e, ub_=ub_, fc=fc, tsl=tsl: e.tensor_tensor(out=actT[:, fc, tsl], in0=sl, in1=ub_, op=ALU.mult),
                                 r=['sl', ukey], w=['actT'])
                wo = w_out[ex].rearrange('(k p) n -> p k n', p=128)
                for cb in range(8):
                    i = woi % 3
                    woi += 1
                    P.dma(lambda e, i=i, cb=cb, wo=wo: e.dma_start(out=wo_bf[i], in_=wo[:, :, cb * 256:(cb + 1) * 256]), w=[wo_bk[i]], q='pool')
                    for t8 in range(8):
                        bk, bkey = self.bank()
                        for fc in range(8):
                            P.op('pe', lambda e, bk=bk, fc=fc, t8=t8, i=i: e.matmul(bk[:, 0:256], lhsT=actT[:, fc, t8 * 128:(t8 + 1) * 128], rhs=wo_bf[i][:, fc, :],
                                                                                 start=(fc == 0), stop=(fc == 7)), r=['actT', wo_bk[i]], w=[bkey])
                        a_ = acc[:, t8, cb * 256:(cb + 1) * 256]
                        cs_ = comb[:, hf * 8 + t8, ex:ex + 1]
                        if ex == 0:
                            P.op('dve', lambda e, bk=bk, a_=a_, cs_=cs_: e.tensor_scalar_mul(out=a_, in0=bk[:, 0:256], scalar1=cs_), r=[bkey, 'comb'], w=['acc'])
                        else:
                            P.op('dve', lambda e, bk=bk, a_=a_, cs_=cs_: e.scalar_tensor_tensor(out=a_, in0=bk[:, 0:256], scalar=cs_, in1=a_, op0=ALU.mult, op1=ALU.add),
                                 r=[bkey, 'comb', 'acc'], w=['acc'])
            for t8 in range(8):
                tt = hf * 8 + t8
                hs, hkk = hs2[t8 % 2], hk2[t8 % 2]
                P.dma(lambda e, hs=hs, tt=tt: e.dma_start(out=hs, in_=hsrc[tt * 128:(tt + 1) * 128, :]), w=[hkk])
                a_ = acc[:, t8, :]
                P.op('dve', lambda e, a_=a_, hs=hs: e.scalar_tensor_tensor(out=a_, in0=hs, scalar=ALPHA, in1=a_, op0=ALU.mult, op1=ALU.add),
                     r=[hkk, 'acc'], w=['acc'])
                self.ln_tile(a_, 'acc', g, b, pk, hs, hkk)
                P.dma(lambda e, hs=hs, tt=tt: e.dma_start(out=hdst[tt * 128:(tt + 1) * 128, :], in_=hs), r=[hkk], w=[self.key('hdst')])
            sb.release(mk2)
            P.barrier()
        self.end_phase(mk)
    def phase_ln(self, lin, hsrc, hdst, ln_g_row, ln_b_row):
        P, sb = self.P, self.sb
        mk = sb.mark()
        g, b, pk = self.ln_params(ln_g_row, ln_b_row)
        ls = [sb.alloc([2048], F32) for _ in range(2)]
        lk = [self.key('lns') for _ in range(2)]
        hs = [sb.alloc([2048], F32) for _ in range(2)]
        hk = [self.key('lnh') for _ in range(2)]
        for tt in range(NT):
            a, ak, h, hkk = ls[tt % 2], lk[tt % 2], hs[tt % 2], hk[tt % 2]
            P.dma(lambda e, a=a, tt=tt: e.dma_start(out=a, in_=lin[tt * 128:(tt + 1) * 128, :]), w=[ak])
            P.dma(lambda e, h=h, tt=tt: e.dma_start(out=h, in_=hsrc[tt * 128:(tt + 1) * 128, :]), w=[hkk])
            P.op('dve', lambda e, a=a, h=h: e.scalar_tensor_tensor(out=a, in0=h, scalar=ALPHA, in1=a, op0=ALU.mult, op1=ALU.add),
                 r=[hkk, ak], w=[ak])
            self.ln_tile(a, ak, g, b, pk, h, hkk)
            P.dma(lambda e, h=h, tt=tt: e.dma_start(out=hdst[tt * 128:(tt + 1) * 128, :], in_=h), r=[hkk], w=[self.key('hdst')])
        self.end_phase(mk)


SB_BYTES = 204800


def build_nc(stop_after=None, dbg=False):
    import math
    import contextlib
    nc = bass.Bass("TRN2", target_bir_lowering=False)
    dr = {}

    def inp(name, shape):
        dr[name] = nc.dram_tensor(name, list(shape), F32, kind="ExternalInput").ap()

    inp('x', (T, D)); inp('mem', (256, D)); inp('w_in', (DEPTH, D, IN_W)); inp('da_lambda', (DEPTH, 4, 64))
    inp('da_norm_g', (DEPTH, 128)); inp('ml_gate_b', (DEPTH, 2, 4)); inp('ml_norm_g', (DEPTH, 128))
    inp('gd_conv_w', (DEPTH, 4, 3072)); inp('gd_a_log', (DEPTH, 8)); inp('gd_dt_bias', (DEPTH, 8)); inp('gd_norm_g', (DEPTH, 128))
    inp('w_out', (DEPTH, D, D)); inp('xa_wq', (DEPTH, D, D)); inp('xa_wkv', (DEPTH, D, 2 * D)); inp('xa_wo', (DEPTH, D, D))
    inp('router_w', (D, 16)); inp('router_bias', (16,)); inp('moe_w_in', (DEPTH, 16, D, D)); inp('moe_w_out', (DEPTH, 16, D // 2, D))
    inp('ln_g', (DEPTH, 3, D)); inp('ln_b', (DEPTH, 3, D))
    inp('ident', (128, 128)); inp('cos', (T, 32)); inp('sin', (T, 32))
    y = nc.dram_tensor('y', [T, D], F32, kind="ExternalOutput").ap()
    okind = "ExternalOutput" if dbg else "Internal"
    proj = nc.dram_tensor('proj', [T, IN_W], F32, kind=okind).ap()
    ymix = nc.dram_tensor('ymix', [T, D], F32, kind=okind).ap()
    lin = nc.dram_tensor('lin', [T, D], F32, kind=okind).ap()
    hres = nc.dram_tensor('hres', [T, D], F32, kind=okind).ap()
    with contextlib.ExitStack() as es:
        pool = es.enter_context(nc.sbuf_tensor("pool", [128, SB_BYTES // 4], F32))
        ps = es.enter_context(nc.psum_tensor("ps", [128, 4096], F32))
        P = Prog(nc)
        sb = SB(pool, SB_BYTES)
        m = MK(nc, P, sb, ps, dr)
        m.consts()
        P.barrier()

        def body():
            hsrc = dr['x']
            for l in range(DEPTH):
                lam_init = 0.8 - 0.6 * math.exp(-0.3 * l)
                mk = sb.mark()
                xT = sb.alloc([KC, T], BF16)
                m.to_featmajor(hsrc, xT, 'xT')
                m.phase_proj(xT, 'xT', dr['w_in'][l], proj, IN_W)
                m.end_phase(mk)
                if stop_after == ('proj', l):
                    return
                mk = sb.mark()
                gens = [m.phase_da(l, proj, ymix, dr['da_lambda'][l], dr['da_norm_g'][l], lam_init),
                        m.phase_ml(l, proj, ymix, dr['ml_gate_b'][l], dr['ml_norm_g'][l])]
                while gens:
                    for g_ in list(gens):
                        try:
                            next(g_)
                        except StopIteration:
                            gens.remove(g_)
                m.end_phase(mk)
                if stop_after == ('ml', l):
                    return
                m.phase_gd(l, proj, ymix, dr['gd_conv_w'][l], dr['gd_a_log'][l], dr['gd_dt_bias'][l], dr['gd_norm_g'][l])
                if stop_after == ('gd', l):
                    return
                mk = sb.mark()
                yT = sb.alloc([KC, T], BF16)
                m.to_featmajor(ymix, yT, 'yT')
                m.phase_proj(yT, 'yT', dr['w_out'][l], lin, D)
                m.end_phase(mk)
                m.phase_ln(lin, hsrc, hres, dr['ln_g'][l, 0], dr['ln_b'][l, 0])
                hsrc = hres
                if stop_after == ('mix', l):
                    return
                mk = sb.mark()
                qT = sb.alloc([KC, T], BF16)
                mk1 = sb.mark()
                xT = sb.alloc([KC, T], BF16)
                m.to_featmajor(hres, xT, 'xT')
                m.phase_xa(l, xT, 'xT', dr['mem'], dr['xa_wq'][l], dr['xa_wkv'][l], qT)
                m.end_phase(mk1)
                m.phase_proj(qT, 'xqT', dr['xa_wo'][l], lin, D)
                m.end_phase(mk)
                m.phase_ln(lin, hres, hres, dr['ln_g'][l, 1], dr['ln_b'][l, 1])
                if stop_after == ('xa', l):
                    return
                hdst = y if l == DEPTH - 1 else hres
                m.phase_moe(l, hres, hdst, dr['router_w'], dr['router_bias'], dr['moe_w_in'][l], dr['moe_w_out'][l],
                            dr['ln_g'][l, 2], dr['ln_b'][l, 2])
                if stop_after == ('moe', l):
                    return

        body()
        P.emit()
    return nc


def host_consts():
    inv = 1.0 / (10000.0 ** (np.arange(0, 64, 2, dtype=np.float32) / 64))
    ang = np.arange(T, dtype=np.float32)[:, None] * inv[None, :].astype(np.float32)
    return {'ident': np.eye(128, dtype=np.float32), 'cos': np.cos(ang).astype(np.float32), 'sin': np.sin(ang).astype(np.float32)}


_NC_CACHE = {}


def kernel(**inputs):
    if 'nc' not in _NC_CACHE:
        _NC_CACHE['nc'] = build_nc()
    nc = _NC_CACHE['nc']
    hc = host_consts()
    shared = {k: np.ascontiguousarray(np.asarray(v, dtype=np.float32)) for k, v in inputs.items() if k not in ('x', 'mem')}
    shared.update(hc)
    x = np.asarray(inputs['x'], dtype=np.float32)
    mem = np.asarray(inputs['mem'], dtype=np.float32)
    in_maps = []
    for c in range(8):
        d = dict(shared)
        d['x'] = np.ascontiguousarray(x[c])
        d['mem'] = np.ascontiguousarray(mem[c])
        in_maps.append(d)
    res = run_bass_kernel_spmd(nc, in_maps, core_ids=list(range(8)))
    return np.stack([np.asarray(r['y']) for r in res.results], axis=0).astype(np.float32)
```

```python
import numpy as np
import concourse.bass as bass
import concourse.mybir as mybir
from concourse.bass_utils import run_bass_kernel_spmd

F32 = mybir.dt.float32
BF16 = mybir.dt.bfloat16
AF = mybir.ActivationFunctionType
ALU = mybir.AluOpType
AX = mybir.AxisListType

COMPUTE = ('pe', 'dve', 'act', 'pool')
NSLOT = 6


class Prog:
    def __init__(self, nc):
        self.nc = nc
        self.streams = {e: [] for e in ('pe', 'dve', 'act', 'pool', 'sp')}
        self.cnt = {e: 0 for e in COMPUTE}
        self.dcnt = {q: 0 for q in ('sp', 'act', 'pool')}
        self.waited = {}
        self.lastw = {}
        self.readers = {}
        self.nwaits = 0

    def _need(self, eng, tok):
        if tok[0] == 'c':
            key = (eng, 'c', tok[1])
            val = tok[2]
            if tok[1] == eng and eng == 'pe':
                return
        else:
            key = (eng, 'd', tok[1], tok[2])
            val = tok[3]
        if self.waited.get(key, 0) >= val:
            return
        self.waited[key] = val
        self.streams[eng].append(('wait', tok))
        self.nwaits += 1

    def _deps(self, eng, r, w):
        toks = []
        for k in list(r) + list(w):
            t = self.lastw.get(k)
            if t is not None:
                toks.append(t)
        for k in w:
            toks.extend(self.readers.get(k, ()))
        for t in toks:
            self._need(eng, t)

    def _commit(self, tok, r, w):
        for k in w:
            self.lastw[k] = tok
            self.readers[k] = []
        for k in r:
            if k in w:
                continue
            self.readers.setdefault(k, []).append(tok)
            if len(self.readers[k]) > 64:
                seen = {}
                for t in self.readers[k]:
                    kk = t[:2] if t[0] == 'c' else t[:3]
                    if kk not in seen or seen[kk][-1] < t[-1]:
                        seen[kk] = t
                self.readers[k] = list(seen.values())

    def op(self, eng, fn, r=(), w=()):
        self._deps(eng, r, w)
        self.cnt[eng] += 1
        tok = ('c', eng, self.cnt[eng])
        self.streams[eng].append(('ins', fn, tok))
        self._commit(tok, r, w)

    def dma(self, fn, r=(), w=(), q='sp'):
        self._deps(q, r, w)
        i = self.dcnt[q]
        self.dcnt[q] += 1
        tok = ('d', q, i % NSLOT, 16 * (i // NSLOT + 1))
        self.streams[q].append(('ins', fn, tok))
        self._commit(tok, r, w)

    def barrier(self):
        toks = [('c', e, self.cnt[e]) for e in COMPUTE if self.cnt[e] > 0]
        for q in self.dcnt:
            n = self.dcnt[q]
            for s in range(NSLOT):
                m = (n - s + NSLOT - 1) // NSLOT if n > s else 0
                if m > 0:
                    toks.append(('d', q, s, 16 * m))
        for e in self.streams:
            for t in toks:
                if t[0] == 'c' and t[1] == e:
                    continue
                self._need(e, t)
        self.lastw = {}
        self.readers = {}

    def emit(self, final_engine='sp'):
        nc = self.nc
        self.barrier()
        import contextlib
        with contextlib.ExitStack() as es:
            csem = {e: es.enter_context(nc.semaphore('c_' + e)) for e in COMPUTE}
            dsem = {(q, s): es.enter_context(nc.semaphore('d_%s%d' % (q, s)))
                    for q in self.dcnt for s in range(NSLOT)}
            block = es.enter_context(nc.Block())

            def run(engname, eh):
                for item in self.streams[engname]:
                    if item[0] == 'wait':
                        t = item[1]
                        if t[0] == 'c':
                            eh.wait_ge(csem[t[1]], t[2])
                        else:
                            eh.wait_ge(dsem[(t[1], t[2])], t[3])
                    else:
                        _, fn, t = item
                        ins = fn(eh)
                        if t[0] == 'c':
                            ins.then_inc(csem[t[1]], 1)
                        else:
                            ins.then_inc(dsem[(t[1], t[2])], 16)

            @block.sync
            def _(e):
                run('sp', e)

            @block.tensor
            def _(e):
                run('pe', e)

            @block.vector
            def _(e):
                run('dve', e)

            @block.scalar
            def _(e):
                run('act', e)

            @block.gpsimd
            def _(e):
                run('pool', e)


class SB:
    def __init__(self, pool_ap, nbytes):
        self.pool = pool_ap
        self.n = nbytes
        self.off = 0
        self.uid = 0

    def mark(self):
        return self.off

    def release(self, m):
        self.off = m

    def alloc(self, shape_free, dt, parts=128):
        esz = 2 if dt == BF16 else 4
        n = int(np.prod(shape_free))
        nb = (n * esz + 31) // 32 * 32
        assert self.off + nb <= self.n, ('SBUF overflow', self.off, nb, self.n)
        a = self.pool[0:parts, self.off // 4:(self.off + nb) // 4]
        self.off += nb
        self.maxoff = max(getattr(self, 'maxoff', 0), self.off)
        if dt != F32:
            a = a.bitcast(dt)
        a = a[:, 0:n]
        if len(shape_free) == 2:
            a = a.rearrange('p (a b) -> p a b', a=shape_free[0])
        elif len(shape_free) == 3:
            a = a.rearrange('p (a b c) -> p a b c', a=shape_free[0], b=shape_free[1])
        return a


T = 2048
D = 2048
NT = 16
KC = 16
DEPTH = 2
IN_W = 7704
ALPHA = (2 * DEPTH) ** 0.25
C_DAQ, C_DAK, C_DAV = 0, 512, 1024
C_MLQ, C_MLK, C_MLV, C_MLO, C_MLI, C_MLF = 1536, 2048, 2560, 3072, 3584, 3588
C_GDQKV, C_GDZ, C_GDA, C_GDB = 3592, 6664, 7688, 7696


class MK:
    def __init__(self, nc, P, sb, ps, dr):
        self.nc, self.P, self.sb, self.ps, self.dr = nc, P, sb, ps, dr
        self.bank_i = 0
        self.uid = 0

    def key(self, s):
        self.uid += 1
        return '%s#%d' % (s, self.uid)

    def bank(self):
        b = self.bank_i % 8
        self.bank_i += 1
        return self.ps[:, b * 512:(b + 1) * 512], 'ps%d' % b

    def consts(self):
        P, sb = self.P, self.sb
        self.ident = sb.alloc([128], F32)
        P.dma(lambda e: e.dma_start(out=self.ident, in_=self.dr['ident']), w=['ident'])
        self.m_ge = sb.alloc([128], F32)
        self.m_gt = sb.alloc([128], F32)
        self.m_lt = sb.alloc([128], F32)
        self.ones = sb.alloc([128], F32)
        self.m_ge_b = sb.alloc([128], BF16)
        self.ones_b = sb.alloc([128], BF16)
        for m, pat, cm, base in ((self.m_ge, 1, -1, 0), (self.m_gt, 1, -1, -1), (self.m_lt, -1, 1, -1)):
            P.op('pool', lambda e, m=m: e.memset(m, 1.0), w=['cmask'])
            P.op('pool', lambda e, m=m, pat=pat, cm=cm, base=base: e.affine_select(
                out=m, in_=m, pattern=[[pat, 128]], compare_op=ALU.is_ge, fill=0.0, base=base,
                channel_multiplier=cm), r=['cmask'], w=['cmask'])
        P.op('pool', lambda e: e.memset(self.ones, 1.0), w=['cmask'])
        P.op('pool', lambda e: e.tensor_copy(out=self.m_ge_b, in_=self.m_ge), r=['cmask'], w=['cmask'])
        P.op('pool', lambda e: e.tensor_copy(out=self.ones_b, in_=self.ones), r=['cmask'], w=['cmask'])
        self.CK = ['ident', 'cmask']

    def to_featmajor(self, src, dstT, dkey, ntt=NT, ncols=2048, hook=None):
        P, sb = self.P, self.sb
        mk = sb.mark()
        stg = [sb.alloc([ncols], F32) for _ in range(2)]
        skey = [self.key('fm_stg') for _ in range(2)]
        nk = ncols // 128
        for tt in range(ntt):
            s, sk = stg[tt % 2], skey[tt % 2]
            P.dma(lambda e, s=s, tt=tt: e.dma_start(out=s, in_=src[tt * 128:(tt + 1) * 128, :]), w=[sk])
            for g in range(nk // 4):
                bk, bkey = self.bank()
                for j in range(4):
                    kc = g * 4 + j
                    P.op('pe', lambda e, bk=bk, j=j, s=s, kc=kc: e.transpose(
                        out=bk[:, j * 128:(j + 1) * 128], in_=s[:, kc * 128:(kc + 1) * 128], identity=self.ident),
                        r=[sk, 'ident'], w=[bkey])
                eng = 'dve' if g % 2 == 0 else 'act'
                dst = dstT[:, g * 4:(g + 1) * 4, tt * 128:(tt + 1) * 128]
                srcp = bk.rearrange('p (a b) -> p a b', a=4)
                if eng == 'dve':
                    P.op('dve', lambda e, dst=dst, srcp=srcp: e.tensor_copy(out=dst, in_=srcp), r=[bkey], w=[dkey])
                else:
                    P.op('act', lambda e, dst=dst, srcp=srcp: e.activation(out=dst, in_=srcp, func=AF.Copy), r=[bkey], w=[dkey])
        sb.release(mk)
        P.barrier()

    def wstream_init(self, kc, ncols, nbuf=2):
        sb = self.sb
        self.ws_bf = [sb.alloc([kc, ncols], BF16) for _ in range(nbuf)]
        self.ws_bk = [self.key('ws_bf') for _ in range(nbuf)]
        self.ws_i = 0
        self.ws_n = nbuf

    def wload(self, wap, c0, ncols, kc, q='pool', cast=None):
        P = self.P
        i = self.ws_i % self.ws_n
        self.ws_i += 1
        bf, bk = self.ws_bf[i], self.ws_bk[i]
        src = wap.rearrange('(k p) n -> p k n', p=128)[:, :, c0:c0 + ncols]
        P.dma(lambda e: e.dma_start(out=bf[:, 0:kc, 0:ncols], in_=src), w=[bk], q='pool')
        return bf, bk

    def phase_proj(self, xT, xkey, w_in, proj, ncols):
        P, sb = self.P, self.sb
        mk = sb.mark()
        self.wstream_init(KC, 512, nbuf=3)
        ost = [sb.alloc([512], F32) for _ in range(4)]
        okey = [self.key('ost') for _ in range(4)]
        oi = 0
        nblk = (ncols + 511) // 512
        for nb in range(nblk):
            c0 = nb * 512
            wd = min(512, ncols - c0)
            wb, wk = self.wload(w_in, c0, wd, KC, cast=('pool' if nb % 2 == 0 else 'act'))
            for tt in range(NT):
                bk, bkey = self.bank()
                for kc in range(KC):
                    P.op('pe', lambda e, bk=bk, kc=kc, tt=tt, wb=wb, wd=wd: e.matmul(
                        bk[:, 0:wd], lhsT=xT[:, kc, tt * 128:(tt + 1) * 128], rhs=wb[:, kc, 0:wd],
                        start=(kc == 0), stop=(kc == KC - 1)), r=[xkey, wk], w=[bkey])
                o, ok = ost[oi % 4], okey[oi % 4]
                oi += 1
                if tt % 2 == 0:
                    P.op('dve', lambda e, o=o, bk=bk, wd=wd: e.tensor_copy(out=o[:, 0:wd], in_=bk[:, 0:wd]), r=[bkey], w=[ok])
                else:
                    P.op('act', lambda e, o=o, bk=bk, wd=wd: e.activation(out=o[:, 0:wd], in_=bk[:, 0:wd], func=AF.Copy), r=[bkey], w=[ok])
                P.dma(lambda e, o=o, tt=tt, c0=c0, wd=wd: e.dma_start(
                    out=proj[tt * 128:(tt + 1) * 128, c0:c0 + wd], in_=o[:, 0:wd]), r=[ok], w=['proj'])
        sb.release(mk)
        P.barrier()

    def ln_params(self, ln_g_row, ln_b_row):
        P, sb = self.P, self.sb
        g = sb.alloc([2048], F32)
        b = sb.alloc([2048], F32)
        k = self.key('lnp')
        P.dma(lambda e: e.dma_start(out=g, in_=ln_g_row.partition_broadcast(128)), w=[k])
        P.dma(lambda e: e.dma_start(out=b, in_=ln_b_row.partition_broadcast(128)), w=[k])
        self.ln_small2 = [sb.alloc([4, 8], F32) for _ in range(2)]
        self.ln_i = 0
        return g, b, k

    def ln_tile(self, acc, akey, g, b, pk, out, okey):
        P = self.P
        sm = self.ln_small2[self.ln_i % 2]
        smk = 'ln_small%d' % (self.ln_i % 2)
        self.ln_i += 1
        st = sm[:, :, 0:6]
        for c in range(4):
            P.op('dve', lambda e, c=c: e.bn_stats(out=sm[:, c, 0:6], in_=acc[:, c * 512:(c + 1) * 512]), r=[akey], w=[smk])
        mv = sm[:, 0, 6:8]
        P.op('dve', lambda e: e.bn_aggr(out=mv, in_=st), r=[smk], w=[smk])
        rstd = sm[:, 1, 6:7]
        nmr = sm[:, 1, 7:8]
        P.op('act', lambda e: e.activation(out=rstd, in_=sm[:, 0, 7:8], func=AF.Sqrt, bias=1e-5), r=[smk], w=[smk])
        P.op('dve', lambda e: e.reciprocal(out=rstd, in_=rstd), r=[smk], w=[smk])
        P.op('dve', lambda e: e.scalar_tensor_tensor(out=nmr, in0=sm[:, 0, 6:7], scalar=-1.0, in1=rstd,
                                                      op0=ALU.mult, op1=ALU.mult), r=[smk], w=[smk])
        P.op('act', lambda e: e.activation(out=out, in_=acc, func=AF.Identity, bias=nmr, scale=rstd),
             r=[akey, smk], w=[okey])
        P.op('pool', lambda e: e.tensor_mul(out=out, in0=out, in1=g), r=[okey, pk], w=[okey])
        P.op('dve', lambda e: e.tensor_add(out=out, in0=out, in1=b), r=[okey, pk], w=[okey])

    def end_phase(self, mk):
        self.sb.release(mk)
        self.P.barrier()

    def rms_gate(self, hsrc, hkey, out, okey, g, gk, scal, tmp, tkey, extra=None):
        P = self.P
        ss = tmp[:, 0:1]
        P.op('act', lambda e: e.activation(out=tmp[:, 8:136], in_=hsrc, func=AF.Square, accum_out=ss), r=[hkey], w=[tkey])
        P.op('act', lambda e: e.activation(out=ss, in_=ss, func=AF.Sqrt, bias=1e-6, scale=1.0 / 128), r=[tkey], w=[tkey])
        P.op('dve', lambda e: e.reciprocal(out=ss, in_=ss), r=[tkey], w=[tkey])
        P.op('dve', lambda e: e.tensor_scalar(out=out, in0=hsrc, scalar1=ss, scalar2=float(scal), op0=ALU.mult, op1=ALU.mult),
             r=[hkey, tkey], w=[okey])
        P.op('pool', lambda e: e.tensor_mul(out=out, in0=out, in1=g), r=[okey, gk], w=[okey])

    def phase_da(self, l, proj, ymix, da_lambda, da_norm_g, lambda_init):
        P, sb = self.P, self.sb
        mk = sb.mark()
        cs = sb.alloc([16, 32], F32)
        sn = sb.alloc([16, 32], F32)
        P.dma(lambda e: e.dma_start(out=cs, in_=self.dr['cos'].rearrange('(t p) j -> p t j', p=128)), w=['cs'])
        P.dma(lambda e: e.dma_start(out=sn, in_=self.dr['sin'].rearrange('(t p) j -> p t j', p=128)), w=['cs'])
        gt = sb.alloc([128], F32)
        P.dma(lambda e: e.dma_start(out=gt, in_=da_norm_g.partition_broadcast(128)), w=['da_g'])
        dl = sb.alloc([4, 64], F32)
        P.dma(lambda e: e.dma_start(out=dl, in_=da_lambda.partition_broadcast(128)), w=['dl'])
        lt = sb.alloc([8], F32)
        pr = sb.alloc([2, 64], F32)
        P.op('dve', lambda e: e.tensor_tensor(out=pr[:, 0, :], in0=dl[:, 0, :], in1=dl[:, 1, :], op=ALU.mult), r=['dl'], w=['pr'])
        P.op('dve', lambda e: e.tensor_tensor(out=pr[:, 1, :], in0=dl[:, 2, :], in1=dl[:, 3, :], op=ALU.mult), r=['dl'], w=['pr'])
        P.op('dve', lambda e: e.reduce_sum(out=lt[:, 0:2], in_=pr, axis=AX.X), r=['pr'], w=['lt'])
        P.op('act', lambda e: e.activation(out=lt[:, 2:4], in_=lt[:, 0:2], func=AF.Exp), r=['lt'], w=['lt'])
        P.op('dve', lambda e: e.tensor_tensor(out=lt[:, 4:5], in0=lt[:, 3:4], in1=lt[:, 2:3], op=ALU.subtract), r=['lt'], w=['lt'])
        P.op('dve', lambda e: e.tensor_scalar_add(out=lt[:, 4:5], in0=lt[:, 4:5], scalar1=-float(lambda_init)), r=['lt'], w=['lt'])
        nlam = lt[:, 4:5]
        qk = sb.alloc([16, 256], F32)
        rot = sb.alloc([16, 256], F32)
        t1 = sb.alloc([16, 32], F32)
        t2 = sb.alloc([16, 32], F32)
        qkT = sb.alloc([2, T], BF16)
        vst = sb.alloc([16, 128], F32)
        vx = sb.alloc([16, 132], BF16)
        yst = sb.alloc([16, 128], F32)
        pT = [sb.alloc([16, 128], BF16) for _ in range(4)]
        pk = [self.key('pT') for _ in range(4)]
        tmp = sb.alloc([136], F32)
        osb = sb.alloc([128], F32)
        pi = 0
        P.op('pool', lambda e: e.memset(vx[:, :, 128:129], 1.0), w=['vx1'])
        for h in range(4):
            P.dma(lambda e, h=h: e.dma_start(out=qk[:, :, 0:128], in_=proj[:, C_DAQ + h * 128:C_DAQ + (h + 1) * 128].rearrange('(t p) d -> p t d', p=128)), w=['qk'])
            P.dma(lambda e, h=h: e.dma_start(out=qk[:, :, 128:256], in_=proj[:, C_DAK + h * 128:C_DAK + (h + 1) * 128].rearrange('(t p) d -> p t d', p=128)), w=['qk'])
            P.dma(lambda e, h=h: e.dma_start(out=vst, in_=proj[:, C_DAV + h * 128:C_DAV + (h + 1) * 128].rearrange('(t p) d -> p t d', p=128)), w=['vst'])
            P.op('pool', lambda e: e.tensor_copy(out=vx[:, :, 0:128], in_=vst), r=['vst'], w=['vx'])
            for gI in range(4):
                a = qk[:, :, gI * 64:gI * 64 + 32]
                b = qk[:, :, gI * 64 + 32:gI * 64 + 64]
                o1 = rot[:, :, gI * 64:gI * 64 + 32]
                o2 = rot[:, :, gI * 64 + 32:gI * 64 + 64]
                eng = 'dve' if gI % 2 == 0 else 'pool'
                tk = 'rt%d' % (gI % 2)
                tt_ = t1 if gI % 2 == 0 else t2
                P.op(eng, lambda e, a=a, o1=o1: e.tensor_tensor(out=o1, in0=a, in1=cs, op=ALU.mult), r=['qk', 'cs'], w=['rot'])
                P.op(eng, lambda e, b=b, tt_=tt_: e.tensor_tensor(out=tt_, in0=b, in1=sn, op=ALU.mult), r=['qk', 'cs'], w=[tk])
                P.op(eng, lambda e, o1=o1, tt_=tt_: e.tensor_tensor(out=o1, in0=o1, in1=tt_, op=ALU.subtract), r=['rot', tk], w=['rot'])
                P.op(eng, lambda e, a=a, o2=o2: e.tensor_tensor(out=o2, in0=a, in1=sn, op=ALU.mult), r=['qk', 'cs'], w=['rot'])
                P.op(eng, lambda e, b=b, tt_=tt_: e.tensor_tensor(out=tt_, in0=b, in1=cs, op=ALU.mult), r=['qk', 'cs', 'rot'], w=[tk])
                P.op(eng, lambda e, o2=o2, tt_=tt_: e.tensor_tensor(out=o2, in0=o2, in1=tt_, op=ALU.add), r=['rot', tk], w=['rot'])
            for which in range(2):
                for g4 in range(4):
                    bk, bkey = self.bank()
                    for j in range(4):
                        tt = g4 * 4 + j
                        P.op('pe', lambda e, bk=bk, j=j, tt=tt, which=which: e.transpose(
                            out=bk[:, j * 128:(j + 1) * 128], in_=rot[:, tt, which * 128:(which + 1) * 128], identity=self.ident),
                            r=['rot', 'ident'], w=[bkey])
                    P.op('act', lambda e, bk=bk, g4=g4, which=which: e.activation(
                        out=qkT[:, which, g4 * 512:(g4 + 1) * 512], in_=bk, func=AF.Copy), r=[bkey], w=['qkT'])
            for qi in range(NT):
                obanks = []
                for c in range(2):
                    p_, pkk = pT[pi % 4], pk[pi % 4]
                    pi += 1
                    nkb = qi + 1
                    for g0 in range(0, nkb, 4):
                        n = min(4, nkb - g0)
                        bk, bkey = self.bank()
                        for j in range(n):
                            kb = g0 + j
                            P.op('pe', lambda e, bk=bk, j=j, kb=kb, c=c, qi=qi: e.matmul(
                                bk[:, j * 128:(j + 1) * 128], lhsT=qkT[c * 64:(c + 1) * 64, 1, kb * 128:(kb + 1) * 128],
                                rhs=qkT[c * 64:(c + 1) * 64, 0, qi * 128:(qi + 1) * 128], start=True, stop=True),
                                r=['qkT'], w=[bkey])
                        P.op('act', lambda e, bk=bk, n=n, g0=g0, p_=p_: e.activation(
                            out=p_[:, g0:g0 + n, :], in_=bk[:, 0:n * 128].rearrange('p (a b) -> p a b', a=n),
                            func=AF.Exp, scale=0.125), r=[bkey], w=[pkk])
                    P.op('pool', lambda e, p_=p_, qi=qi: e.tensor_mul(out=p_[:, qi, :], in0=p_[:, qi, :], in1=self.m_ge_b),
                         r=[pkk, 'cmask'], w=[pkk])
                    ob, obkey = self.bank()
                    for kb in range(nkb):
                        P.op('pe', lambda e, ob=ob, kb=kb, p_=p_, nkb=nkb: e.matmul(
                            ob[:, 0:129], lhsT=p_[:, kb, :], rhs=vx[:, kb, 0:129], start=(kb == 0), stop=(kb == nkb - 1)),
                            r=[pkk, 'vx', 'vx1'], w=[obkey])
                    obanks.append((ob, obkey))
                (o0, k0), (o1_, k1) = obanks
                P.op('dve', lambda e, o0=o0: e.reciprocal(out=tmp[:, 1:2], in_=o0[:, 128:129]), r=[k0], w=['datmp'])
                P.op('dve', lambda e, o1_=o1_: e.reciprocal(out=tmp[:, 2:3], in_=o1_[:, 128:129]), r=[k1], w=['datmp'])
                P.op('dve', lambda e: e.tensor_tensor(out=tmp[:, 2:3], in0=tmp[:, 2:3], in1=nlam, op=ALU.mult), r=['datmp', 'lt'], w=['datmp'])
                P.op('dve', lambda e, o0=o0: e.tensor_scalar_mul(out=osb, in0=o0[:, 0:128], scalar1=tmp[:, 1:2]), r=[k0, 'datmp'], w=['osb'])
                P.op('dve', lambda e, o1_=o1_: e.scalar_tensor_tensor(out=osb, in0=o1_[:, 0:128], scalar=tmp[:, 2:3], in1=osb,
                                                                       op0=ALU.mult, op1=ALU.add), r=[k1, 'datmp', 'osb'], w=['osb'])
                self.rms_gate(osb, 'osb', yst[:, qi, :], 'yst', gt, 'da_g', 1.0 - lambda_init, tmp, 'datmp')
                yield
            P.dma(lambda e, h=h: e.dma_start(out=ymix[:, h * 128:(h + 1) * 128].rearrange('(t p) d -> p t d', p=128), in_=yst),
                  r=['yst'], w=[self.key('ymix')])

    def phase_ml(self, l, proj, ymix, ml_gate_b, ml_norm_g):
        P, sb = self.P, self.sb
        mk = sb.mark()
        gt = sb.alloc([128], F32)
        P.dma(lambda e: e.dma_start(out=gt, in_=ml_norm_g.partition_broadcast(128)), w=['ml_g'])
        gb = sb.alloc([2, 4], F32)
        P.dma(lambda e: e.dma_start(out=gb, in_=ml_gate_b.partition_broadcast(128)), w=['gb'])
        gi = sb.alloc([16, 4], F32)
        gf = sb.alloc([16, 4], F32)
        P.dma(lambda e: e.dma_start(out=gi, in_=proj[:, C_MLI:C_MLI + 4].rearrange('(t p) d -> p t d', p=128)), w=['gi'])
        P.dma(lambda e: e.dma_start(out=gf, in_=proj[:, C_MLF:C_MLF + 4].rearrange('(t p) d -> p t d', p=128)), w=['gf'])
        P.op('dve', lambda e: e.tensor_tensor(out=gi, in0=gi, in1=gb[:, 0:1, :].to_broadcast([128, 16, 4]), op=ALU.add), r=['gi', 'gb'], w=['gi'])
        P.op('dve', lambda e: e.tensor_tensor(out=gf, in0=gf, in1=gb[:, 1:2, :].to_broadcast([128, 16, 4]), op=ALU.add), r=['gf', 'gb'], w=['gf'])
        P.op('act', lambda e: e.activation(out=gf, in_=gf, func=AF.Exp, scale=-1.0), r=['gf'], w=['gf'])
        P.op('act', lambda e: e.activation(out=gf, in_=gf, func=AF.Ln, bias=1.0), r=['gf'], w=['gf'])
        P.op('dve', lambda e: e.tensor_scalar_mul(out=gf, in0=gf, scalar1=-1.0), r=['gf'], w=['gf'])
        gf2 = gf.rearrange('p a b -> p (a b)')
        gi2 = gi.rearrange('p a b -> p (a b)')
        aq = sb.alloc([16, 4], F32)
        ak = sb.alloc([16, 4], F32)
        ak2 = sb.alloc([16, 4], F32)
        dec = sb.alloc([16, 4], F32)
        bk, bkey = self.bank()
        P.op('pe', lambda e: e.matmul(bk[:, 0:64], lhsT=self.m_ge, rhs=gf2, start=True, stop=True), r=['gf', 'cmask'], w=[bkey])
        P.op('pe', lambda e: e.matmul(bk[:, 64:128], lhsT=self.ones, rhs=gf2, start=True, stop=True), r=['gf', 'cmask'], w=[bkey])
        aq2, ak_2, ak22, dec2 = (x.rearrange('p a b -> p (a b)') for x in (aq, ak, ak2, dec))
        bsb = sb.alloc([128], F32)
        P.op('act', lambda e: e.activation(out=bsb, in_=bk[:, 0:128], func=AF.Copy), r=[bkey], w=['bsb'])
        P.op('act', lambda e: e.activation(out=aq2, in_=bsb[:, 0:64], func=AF.Exp), r=['bsb'], w=['mlg'])
        P.op('act', lambda e: e.activation(out=dec2, in_=bsb[:, 64:128], func=AF.Exp), r=['bsb'], w=['mlg'])
        P.op('dve', lambda e: e.tensor_tensor(out=ak_2, in0=gi2, in1=bsb[:, 0:64], op=ALU.subtract), r=['gi', 'bsb'], w=['mlg'])
        P.op('dve', lambda e: e.tensor_tensor(out=ak22, in0=ak_2, in1=bsb[:, 64:128], op=ALU.add), r=['bsb', 'mlg'], w=['mlg'])
        P.op('act', lambda e: e.activation(out=ak_2, in_=ak_2, func=AF.Exp), r=['mlg'], w=['mlg'])
        P.op('act', lambda e: e.activation(out=ak22, in_=ak22, func=AF.Exp), r=['mlg'], w=['mlg'])
        qs = sb.alloc([16, 128], F32)
        ks = sb.alloc([16, 128], F32)
        vs = sb.alloc([16, 128], F32)
        osg = sb.alloc([16, 128], F32)
        k2 = sb.alloc([16, 128], BF16)
        vx = sb.alloc([16, 132], BF16)
        qT = sb.alloc([T], BF16)
        kT = sb.alloc([T], BF16)
        yst = sb.alloc([16, 128], F32)
        pTs = [sb.alloc([128], BF16) for _ in range(2)]
        pks = [self.key('mlp') for _ in range(2)]
        M = sb.alloc([132], F32)
        Mb = sb.alloc([132], BF16)
        tmp = sb.alloc([136], F32)
        hsb = sb.alloc([128], F32)
        P.op('pool', lambda e: e.memset(vx[:, :, 128:129], 1.0), w=['mvx1'])
        sc = 128 ** -0.5
        for h in range(4):
            for dst, c0, kk in ((qs, C_MLQ, 'mlq'), (ks, C_MLK, 'mlk'), (vs, C_MLV, 'mlv'), (osg, C_MLO, 'mlo')):
                P.dma(lambda e, dst=dst, c0=c0, h=h: e.dma_start(
                    out=dst, in_=proj[:, c0 + h * 128:c0 + (h + 1) * 128].rearrange('(t p) d -> p t d', p=128)), w=[kk])
            P.op('act', lambda e: e.activation(out=osg, in_=osg, func=AF.Sigmoid), r=['mlo'], w=['mlo'])
            P.op('pool', lambda e: e.tensor_copy(out=vx[:, :, 0:128], in_=vs), r=['mlv'], w=['mvx'])
            P.op('dve', lambda e, h=h: e.tensor_tensor(out=qs, in0=qs, in1=aq[:, :, h:h + 1].to_broadcast([128, 16, 128]), op=ALU.mult), r=['mlq', 'mlg'], w=['mlq'])
            P.op('dve', lambda e, h=h: e.scalar_tensor_tensor(out=k2, in0=ks, scalar=sc, in1=ak2[:, :, h:h + 1].to_broadcast([128, 16, 128]),
                                                               op0=ALU.mult, op1=ALU.mult), r=['mlk', 'mlg'], w=['k2'])
            P.op('dve', lambda e, h=h: e.scalar_tensor_tensor(out=ks, in0=ks, scalar=sc, in1=ak[:, :, h:h + 1].to_broadcast([128, 16, 128]),
                                                                op0=ALU.mult, op1=ALU.mult), r=['mlk', 'mlg', 'k2'], w=['mlk'])
            for src, skey, dstT, dk in ((qs, 'mlq', qT, 'qT'), (ks, 'mlk', kT, 'kT')):
                for g4 in range(4):
                    bk, bkey = self.bank()
                    for j in range(4):
                        tt = g4 * 4 + j
                        P.op('pe', lambda e, bk=bk, j=j, tt=tt, src=src: e.transpose(
                            out=bk[:, j * 128:(j + 1) * 128], in_=src[:, tt, :], identity=self.ident), r=[skey, 'ident'], w=[bkey])
                    P.op('act', lambda e, bk=bk, g4=g4, dstT=dstT: e.activation(out=dstT[:, g4 * 512:(g4 + 1) * 512], in_=bk, func=AF.Copy),
                         r=[bkey], w=[dk])
            for c in range(NT):
                sl = slice(c * 128, (c + 1) * 128)
                p_, pkk = pTs[c % 2], pks[c % 2]
                bk, bkey = self.bank()
                P.op('pe', lambda e, bk=bk, sl=sl: e.matmul(bk[:, 0:128], lhsT=kT[:, sl], rhs=qT[:, sl], start=True, stop=True),
                     r=['qT', 'kT'], w=[bkey])
                P.op('dve', lambda e, bk=bk, p_=p_: e.tensor_tensor(out=p_, in0=bk[:, 0:128], in1=self.m_ge, op=ALU.mult),
                     r=[bkey, 'cmask'], w=[pkk])
                nb, nkey = self.bank()
                P.op('pe', lambda e, nb=nb, p_=p_, c=c: e.matmul(nb[:, 0:129], lhsT=p_, rhs=vx[:, c, 0:129], start=True, stop=(c == 0)),
                     r=[pkk, 'mvx', 'mvx1'], w=[nkey])
                if c > 0:
                    P.op('pe', lambda e, nb=nb, sl=sl: e.matmul(nb[:, 0:129], lhsT=qT[:, sl], rhs=Mb[:, 0:129], start=False, stop=True),
                         r=['qT', 'Mb'], w=[nkey])
                if c < NT - 1:
                    sbk, skey = self.bank()
                    P.op('pe', lambda e, sbk=sbk, c=c: e.matmul(sbk[:, 0:129], lhsT=k2[:, c, :], rhs=vx[:, c, 0:129], start=True, stop=True),
                         r=['k2', 'mvx', 'mvx1'], w=[skey])
                    if c == 0:
                        P.op('dve', lambda e, sbk=sbk: e.tensor_copy(out=M[:, 0:129], in_=sbk[:, 0:129]), r=[skey], w=['M'])
                    else:
                        P.op('dve', lambda e, sbk=sbk, c=c, h=h: e.scalar_tensor_tensor(
                            out=M[:, 0:129], in0=M[:, 0:129], scalar=dec[:, c, h:h + 1], in1=sbk[:, 0:129], op0=ALU.mult, op1=ALU.add),
                            r=[skey, 'M', 'mlg'], w=['M'])
                    P.op('act', lambda e: e.activation(out=Mb[:, 0:129], in_=M[:, 0:129], func=AF.Copy), r=['M'], w=['Mb'])
                P.op('dve', lambda e, nb=nb: e.tensor_copy(out=tmp[:, 3:4], in_=nb[:, 128:129]), r=[nkey], w=['mltmp'])
                P.op('dve', lambda e: e.scalar_tensor_tensor(out=tmp[:, 1:2], in0=tmp[:, 3:4], scalar=-1.0, in1=tmp[:, 3:4],
                                                             op0=ALU.mult, op1=ALU.max), r=['mltmp'], w=['mltmp'])
                P.op('dve', lambda e: e.tensor_scalar_max(out=tmp[:, 1:2], in0=tmp[:, 1:2], scalar1=1.0), r=['mltmp'], w=['mltmp'])
                P.op('dve', lambda e: e.reciprocal(out=tmp[:, 1:2], in_=tmp[:, 1:2]), r=['mltmp'], w=['mltmp'])
                P.op('dve', lambda e, nb=nb: e.tensor_scalar_mul(out=hsb, in0=nb[:, 0:128], scalar1=tmp[:, 1:2]), r=[nkey, 'mltmp'], w=['hsb'])
                self.rms_gate(hsb, 'hsb', yst[:, c, :], 'myst', gt, 'ml_g', 1.0, tmp, 'mltmp')
                yield
            P.op('pool', lambda e: e.tensor_mul(out=yst, in0=yst, in1=osg), r=['myst', 'mlo'], w=['myst'])
            P.dma(lambda e, h=h: e.dma_start(out=ymix[:, 512 + h * 128:512 + (h + 1) * 128].rearrange('(t p) d -> p t d', p=128), in_=yst),
                  r=['myst'], w=[self.key('ymix')])

    def phase_gd(self, l, proj, ymix, conv_w, a_log, dt_bias, gd_norm_g):
        P, sb = self.P, self.sb
        mk = sb.mark()
        TM = lambda c0, h: proj[:, c0 + h * 128:c0 + (h + 1) * 128].rearrange('(t p) d -> p t d', p=128)
        gt = sb.alloc([128], F32)
        P.dma(lambda e: e.dma_start(out=gt, in_=gd_norm_g.partition_broadcast(128)), w=['gd_g'])
        al = sb.alloc([1, 8], F32)
        db = sb.alloc([1, 8], F32)
        P.dma(lambda e: e.dma_start(out=al[:, 0, :], in_=a_log.partition_broadcast(128)), w=['al'])
        P.dma(lambda e: e.dma_start(out=db[:, 0, :], in_=dt_bias.partition_broadcast(128)), w=['db'])
        ga = sb.alloc([16, 8], F32)
        beta = sb.alloc([16, 8], F32)
        t3 = sb.alloc([16, 8], F32)
        P.dma(lambda e: e.dma_start(out=ga, in_=proj[:, C_GDA:C_GDA + 8].rearrange('(t p) d -> p t d', p=128)), w=['ga'])
        P.dma(lambda e: e.dma_start(out=beta, in_=proj[:, C_GDB:C_GDB + 8].rearrange('(t p) d -> p t d', p=128)), w=['beta'])
        P.op('act', lambda e: e.activation(out=beta, in_=beta, func=AF.Sigmoid), r=['beta'], w=['beta'])
        P.op('act', lambda e: e.activation(out=al, in_=al, func=AF.Exp), r=['al'], w=['al'])
        P.op('dve', lambda e: e.tensor_tensor(out=ga, in0=ga, in1=db[:, 0:1, :].to_broadcast([128, 16, 8]), op=ALU.add), r=['ga', 'db'], w=['ga'])
        P.op('dve', lambda e: e.scalar_tensor_tensor(out=t3, in0=ga, scalar=-1.0, in1=ga, op0=ALU.mult, op1=ALU.max), r=['ga'], w=['t3'])
        P.op('act', lambda e: e.activation(out=t3, in_=t3, func=AF.Exp, scale=-1.0), r=['t3'], w=['t3'])
        P.op('act', lambda e: e.activation(out=t3, in_=t3, func=AF.Ln, bias=1.0), r=['t3'], w=['t3'])
        P.op('dve', lambda e: e.tensor_scalar_max(out=ga, in0=ga, scalar1=0.0), r=['ga'], w=['ga'])
        P.op('dve', lambda e: e.tensor_add(out=ga, in0=ga, in1=t3), r=['ga', 't3'], w=['ga'])
        P.op('dve', lambda e: e.scalar_tensor_tensor(out=ga, in0=ga, scalar=-1.0, in1=al[:, 0:1, :].to_broadcast([128, 16, 8]),
                                                      op0=ALU.mult, op1=ALU.mult), r=['ga', 'al'], w=['ga'])
        ga2 = ga.rearrange('p a b -> p (a b)')
        egc = sb.alloc([16, 8], F32)
        ekg = sb.alloc([16, 8], F32)
        egl = sb.alloc([16, 8], F32)
        bk, bkey = self.bank()
        P.op('pe', lambda e: e.matmul(bk[:, 0:128], lhsT=self.m_ge, rhs=ga2, start=True, stop=True), r=['ga', 'cmask'], w=[bkey])
        P.op('pe', lambda e: e.matmul(bk[:, 128:256], lhsT=self.ones, rhs=ga2, start=True, stop=True), r=['ga', 'cmask'], w=[bkey])
        f2 = lambda x: x.rearrange('p a b -> p (a b)')
        P.op('act', lambda e: e.activation(out=f2(egc), in_=bk[:, 0:128], func=AF.Exp), r=[bkey], w=['gdg'])
        P.op('act', lambda e: e.activation(out=f2(egl), in_=bk[:, 128:256], func=AF.Exp), r=[bkey], w=['gdg'])
        P.op('act', lambda e: e.activation(out=f2(t3), in_=bk[:, 0:128], func=AF.Copy), r=[bkey, 't3'], w=['t3'])
        P.op('dve', lambda e: e.tensor_tensor(out=f2(ekg), in0=bk[:, 128:256], in1=f2(t3), op=ALU.subtract), r=[bkey, 't3'], w=['ekg'])
        P.op('act', lambda e: e.activation(out=f2(ekg), in_=f2(ekg), func=AF.Exp), r=['ekg'], w=['ekg'])
        import os
        GS = os.environ.get('GD_STOP', '')
        if GS == 'gates':
            self.end_phase(mk)
            return
        xs = [sb.alloc([16, 128], F32) for _ in range(4)]
        cw = sb.alloc([4, 3, 128], F32)
        cv = [sb.alloc([16, 128], F32) for _ in range(3)]
        ckey = ['gq', 'gk', 'gv']
        sq = sb.alloc([16, 128], F32)
        ssq = sb.alloc([16, 1], F32)
        kbeta = sb.alloc([16, 128], F32)
        vb = sb.alloc([16, 128], F32)
        kbg = sb.alloc([16, 128], F32)
        qg = sb.alloc([16, 128], F32)
        BS = [(sb.alloc([16, 128], F32), sb.alloc([16, 128], BF16), sb.alloc([16, 128], BF16), sb.alloc([16, 128], BF16), sb.alloc([16, 128], BF16)) for _ in range(2)]
        zt = sb.alloc([16, 128], F32)
        G = 4
        TR = sb.alloc([G, 4, 128], F32)
        kTt = TR[:, :, 0, :]
        kbTt = TR[:, :, 1, :]
        qTt = TR[:, :, 2, :]
        Et = sb.alloc([G, 128], F32)
        utg = sb.alloc([G, 128], F32)
        Bt = [sb.alloc([G, 128], F32) for _ in range(2)]
        Nt = [sb.alloc([G, 128], F32) for _ in range(2)]
        Rt = [sb.alloc([G, 128], F32) for _ in range(2)]
        S = sb.alloc([128], F32)
        Sb = sb.alloc([128], BF16)
        vn = sb.alloc([128], BF16)
        yst = sb.alloc([16, 128], F32)
        tmp = sb.alloc([136], F32)
        osb = sb.alloc([128], F32)
        for x_ in xs[0:3]:
            P.op('pool', lambda e, x_=x_: e.memset(x_[0:32, 0, :], 0.0), w=['xs'])
        sc = 128 ** -0.5
        def front(h, sidx):
            U, WT, AT, QGT, kgs = BS[sidx]
            kU, kWT, kAT, kQGT, kkgs = ('%s%d' % (n_, sidx) for n_ in ('U', 'WT', 'AT', 'QGT', 'kgs'))
            for j in range(3):
                P.dma(lambda e, h=h, j=j: e.dma_start(
                    out=cw[:, :, j, :], in_=conv_w[:, j * 1024 + h * 128:j * 1024 + (h + 1) * 128].partition_broadcast(128)), w=['cw'])
            for j in range(3):
                src = TM(C_GDQKV + j * 1024, h)
                xk = 'xs%d' % j
                for i in range(4):
                    s_ = 3 - i
                    if s_ == 0:
                        P.dma(lambda e, src=src: e.dma_start(out=xs[3], in_=src), r=['xs'], w=[xk])
                    else:
                        P.dma(lambda e, src=src, i=i, s_=s_: e.dma_start(out=xs[i][s_:128, :, :], in_=src[0:128 - s_, :, :]), r=['xs'], w=[xk])
                        P.dma(lambda e, src=src, i=i, s_=s_: e.dma_start(out=xs[i][0:s_, 1:16, :], in_=src[128 - s_:128, 0:15, :]), r=['xs'], w=[xk])
                acc = cv[j]
                eng = 'dve' if j != 1 else 'pool'
                wvs = [cw[:, i:i + 1, j, :].to_broadcast([128, 16, 128]) for i in range(4)]
                P.op(eng, lambda e, acc=acc, w3=wvs[3]: e.tensor_tensor(out=acc, in0=xs[3], in1=w3, op=ALU.mult), r=[xk, 'cw'], w=[ckey[j]])
                for i in range(3):
                    P.op(eng, lambda e, i=i, wi_=wvs[i]: e.tensor_tensor(out=xs[i], in0=xs[i], in1=wi_, op=ALU.mult), r=[xk, 'cw'], w=[xk])
                    P.op(eng, lambda e, i=i, acc=acc: e.tensor_add(out=acc, in0=acc, in1=xs[i]), r=[xk, ckey[j]], w=[ckey[j]])
                P.op('act', lambda e, acc=acc: e.activation(out=acc, in_=acc, func=AF.Silu), r=[ckey[j]], w=[ckey[j]])
                P.op('pool', lambda e: e.memset(xs[0][0:32, 0, :], 0.0), w=['xs', xk])
                P.op('pool', lambda e: e.memset(xs[1][0:32, 0, :], 0.0), w=['xs', xk])
                P.op('pool', lambda e: e.memset(xs[2][0:32, 0, :], 0.0), w=['xs', xk])
                if j < 2:
                    P.op('pool', lambda e, acc=acc: e.tensor_mul(out=sq, in0=acc, in1=acc), r=[ckey[j]], w=['sq'])
                    P.op('dve', lambda e: e.reduce_sum(out=ssq[:, :, 0], in_=sq, axis=AX.X), r=['sq'], w=['ssq'])
                    P.op('act', lambda e: e.activation(out=ssq, in_=ssq, func=AF.Sqrt, bias=1e-6), r=['ssq'], w=['ssq'])
                    P.op('dve', lambda e: e.reciprocal(out=ssq, in_=ssq), r=['ssq'], w=['ssq'])
                    P.op('dve', lambda e, acc=acc, j=j: e.scalar_tensor_tensor(
                        out=acc, in0=acc, scalar=(sc if j == 0 else 1.0), in1=ssq.to_broadcast([128, 16, 128]),
                        op0=ALU.mult, op1=ALU.mult), r=[ckey[j], 'ssq'], w=[ckey[j]])
                yield
            qn, kn, vv = cv
            bc = lambda t_, h=h: t_[:, :, h:h + 1].to_broadcast([128, 16, 128])
            b_beta, b_egc, b_ekg = bc(beta), bc(egc), bc(ekg)
            P.op('dve', lambda e, b_beta=b_beta: e.tensor_tensor(out=kbeta, in0=kn, in1=b_beta, op=ALU.mult), r=['gk', 'beta'], w=['kbeta'])
            P.op('pool', lambda e, b_beta=b_beta: e.tensor_tensor(out=vb, in0=vv, in1=b_beta, op=ALU.mult), r=['gv', 'beta'], w=['vb'])
            P.op('dve', lambda e, b_egc=b_egc: e.tensor_tensor(out=kbg, in0=kbeta, in1=b_egc, op=ALU.mult), r=['kbeta', 'gdg'], w=['kbg'])
            P.op('pool', lambda e, b_egc=b_egc: e.tensor_tensor(out=qg, in0=qn, in1=b_egc, op=ALU.mult), r=['gq', 'gdg'], w=['qg'])
            P.op('dve', lambda e, b_ekg=b_ekg: e.tensor_tensor(out=kgs, in0=kn, in1=b_ekg, op=ALU.mult), r=['gk', 'ekg'], w=[kkgs])
            yield
            for c0 in range(0, NT, G):
                tiles = list(range(c0, c0 + G))
                gk_ = lambda s, i: '%s_%d' % (s, i)
                for i, c in enumerate(tiles):
                    bk, bkey = self.bank()
                    for j, (src, skey) in enumerate(((kn, 'gk'), (kbeta, 'kbeta'), (qn, 'gq'), (qg, 'qg'))):
                        P.op('pe', lambda e, bk=bk, j=j, src=src, c=c: e.transpose(out=bk[:, j * 128:(j + 1) * 128], in_=src[:, c, :], identity=self.ident),
                             r=[skey, 'ident'], w=[bkey])
                    ev = 'act' if i % 2 == 0 else 'dve'
                    if ev == 'act':
                        P.op('act', lambda e, bk=bk, i=i: e.activation(out=TR[:, i, :, :], in_=bk.rearrange('p (a b) -> p a b', a=4), func=AF.Copy),
                             r=[bkey], w=[gk_('kT', i), gk_('kbT', i), gk_('qT', i), gk_('qgT', i)])
                    else:
                        P.op('dve', lambda e, bk=bk, i=i: e.tensor_copy(out=TR[:, i, :, :], in_=bk.rearrange('p (a b) -> p a b', a=4)),
                             r=[bkey], w=[gk_('kT', i), gk_('kbT', i), gk_('qT', i), gk_('qgT', i)])
                    P.op('pool', lambda e, i=i, c=c: e.tensor_copy(out=QGT[:, c, :], in_=TR[:, i, 3, :]), r=[gk_('qgT', i)], w=[kQGT])
                    GV = ''
                    if GV != 'noutg':
                        P.op('dve', lambda e, i=i, c=c, h=h: e.tensor_scalar_mul(out=utg[:, i, :], in0=self.m_ge, scalar1=ga[:, c, h:h + 1]),
                             r=['cmask', 'ga'], w=[gk_('utg', i)])
                yield
                banks = []
                for i, c in enumerate(tiles):
                    bk, bkey = self.bank()
                    banks.append((bk, bkey))
                    P.op('pe', lambda e, bk=bk, i=i: e.matmul(bk[:, 0:128], lhsT=self.m_lt, rhs=utg[:, i, :], start=True, stop=True),
                         r=['cmask', gk_('utg', i)], w=[bkey])
                    P.op('pe', lambda e, bk=bk, i=i: e.matmul(bk[:, 128:256], lhsT=kTt[:, i, :], rhs=kbTt[:, i, :], start=True, stop=True),
                         r=[gk_('kT', i), gk_('kbT', i)], w=[bkey])
                    P.op('pe', lambda e, bk=bk, i=i: e.matmul(bk[:, 256:384], lhsT=kTt[:, i, :], rhs=qTt[:, i, :], start=True, stop=True),
                         r=[gk_('kT', i), gk_('qT', i)], w=[bkey])
                for i, c in enumerate(tiles):
                    bk, bkey = banks[i]
                    P.op('act', lambda e, bk=bk, i=i: e.activation(out=Et[:, i, :], in_=bk[:, 0:128], func=AF.Exp), r=[bkey], w=[gk_('E', i)])
                    P.op('dve', lambda e, bk=bk, i=i: e.scalar_tensor_tensor(out=Bt[0][:, i, :], in0=bk[:, 128:256], scalar=-1.0, in1=Et[:, i, :],
                                                                              op0=ALU.mult, op1=ALU.mult), r=[bkey, gk_('E', i)], w=[gk_('B0', i)])
                    P.op('pool', lambda e, i=i: e.tensor_mul(out=Bt[0][:, i, :], in0=Bt[0][:, i, :], in1=self.m_gt), r=[gk_('B0', i), 'cmask'], w=[gk_('B0', i)])
                    P.op('dve', lambda e, bk=bk, i=i: e.tensor_tensor(out=Et[:, i, :], in0=bk[:, 256:384], in1=Et[:, i, :], op=ALU.mult),
                         r=[bkey, gk_('E', i)], w=[gk_('E', i)])
                    P.op('pool', lambda e, i=i, c=c: e.tensor_mul(out=AT[:, c, :], in0=Et[:, i, :], in1=self.m_ge), r=[gk_('E', i), 'cmask'], w=[kAT])
                yield
                banks = []
                for i, c in enumerate(tiles):
                    bk, bkey = self.bank()
                    banks.append((bk, bkey))
                    P.op('pe', lambda e, bk=bk, i=i: e.transpose(out=bk[:, 0:128], in_=Bt[0][:, i, :], identity=self.ident),
                         r=[gk_('B0', i), 'ident'], w=[bkey])
                for i, c in enumerate(tiles):
                    bk, bkey = banks[i]
                    P.op('act', lambda e, bk=bk, i=i: e.activation(out=Nt[0][:, i, :], in_=bk[:, 0:128], func=AF.Copy), r=[bkey], w=[gk_('N0', i)])
                    P.op('dve', lambda e, i=i: e.tensor_add(out=Rt[0][:, i, :], in0=Bt[0][:, i, :], in1=self.ident), r=[gk_('B0', i), 'ident'], w=[gk_('R0', i)])
                yield
                NLEV = 6
                for lev in range(NLEV):
                    a, b = lev % 2, (lev + 1) % 2
                    last = (lev == NLEV - 1)
                    yield
                    banks = []
                    for i, c in enumerate(tiles):
                        bk, bkey = self.bank()
                        banks.append((bk, bkey))
                        P.op('pe', lambda e, bk=bk, i=i, a=a: e.matmul(bk[:, 0:128], lhsT=Bt[a][:, i, :], rhs=Nt[a][:, i, :], start=True, stop=True),
                             r=[gk_('B%d' % a, i), gk_('N%d' % a, i)], w=[bkey])
                        if not last:
                            P.op('pe', lambda e, bk=bk, i=i, a=a: e.matmul(bk[:, 128:256], lhsT=Nt[a][:, i, :], rhs=Bt[a][:, i, :], start=True, stop=True),
                                 r=[gk_('B%d' % a, i), gk_('N%d' % a, i)], w=[bkey])
                    yield
                    for i, c in enumerate(tiles):
                        bk, bkey = banks[i]
                        if i % 2 == 0:
                            P.op('act', lambda e, bk=bk, i=i, b=b: e.activation(out=Nt[b][:, i, :], in_=bk[:, 0:128], func=AF.Copy), r=[bkey], w=[gk_('N%d' % b, i)])
                            if not last:
                                P.op('act', lambda e, bk=bk, i=i, b=b: e.activation(out=Bt[b][:, i, :], in_=bk[:, 128:256], func=AF.Copy), r=[bkey], w=[gk_('B%d' % b, i)])
                        else:
                            P.op('dve', lambda e, bk=bk, i=i, b=b: e.tensor_copy(out=Nt[b][:, i, :], in_=bk[:, 0:128]), r=[bkey], w=[gk_('N%d' % b, i)])
                            if not last:
                                P.op('dve', lambda e, bk=bk, i=i, b=b: e.tensor_copy(out=Bt[b][:, i, :], in_=bk[:, 128:256]), r=[bkey], w=[gk_('B%d' % b, i)])
                    yield
                    for i, c in enumerate(tiles):
                        bk, bkey = banks[i]
                        P.op('pe', lambda e, bk=bk, i=i, a=a, b=b: e.matmul(bk[:, 256:384], lhsT=Nt[b][:, i, :], rhs=Rt[a][:, i, :], start=True, stop=True),
                             r=[gk_('N%d' % b, i), gk_('R%d' % a, i)], w=[bkey])
                    yield
                    for i, c in enumerate(tiles):
                        bk, bkey = banks[i]
                        P.op('dve', lambda e, bk=bk, i=i, a=a, b=b: e.tensor_tensor(out=Rt[b][:, i, :], in0=bk[:, 256:384], in1=Rt[a][:, i, :], op=ALU.add),
                             r=[bkey, gk_('R%d' % a, i)], w=[gk_('R%d' % b, i)])
                yield
                rf = NLEV % 2
                for i, c in enumerate(tiles):
                    bk, bkey = self.bank()
                    P.op('pe', lambda e, bk=bk, i=i, c=c: e.matmul(bk[:, 0:128], lhsT=Rt[rf][:, i, :], rhs=vb[:, c, :], start=True, stop=True),
                         r=[gk_('R%d' % rf, i), 'vb'], w=[bkey])
                    P.op('pe', lambda e, bk=bk, i=i, c=c: e.matmul(bk[:, 128:256], lhsT=kbg[:, c, :], rhs=Rt[rf][:, i, :], start=True, stop=True),
                         r=[gk_('R%d' % rf, i), 'kbg'], w=[bkey])
                    P.op('dve', lambda e, bk=bk, c=c: e.tensor_copy(out=U[:, c, :], in_=bk[:, 0:128]), r=[bkey], w=[kU])
                    P.op('dve', lambda e, bk=bk, c=c: e.tensor_copy(out=WT[:, c, :], in_=bk[:, 128:256]), r=[bkey], w=[kWT])
        def back(h, sidx):
            U, WT, AT, QGT, kgs = BS[sidx]
            kU, kWT, kAT, kQGT, kkgs = ('%s%d' % (n_, sidx) for n_ in ('U', 'WT', 'AT', 'QGT', 'kgs'))
            P.dma(lambda e, h=h: e.dma_start(out=zt, in_=TM(C_GDZ, h)), w=['zt'])
            P.op('act', lambda e: e.activation(out=zt, in_=zt, func=AF.Silu), r=['zt'], w=['zt'])
            for c in range(NT):
                if c == 0:
                    P.op('dve', lambda e: e.tensor_copy(out=vn, in_=U[:, 0, :]), r=[kU], w=['vn'])
                else:
                    bk, bkey = self.bank()
                    P.op('pe', lambda e, bk=bk, c=c: e.matmul(bk[:, 0:128], lhsT=WT[:, c, :], rhs=Sb, start=True, stop=True), r=[kWT, 'Sb'], w=[bkey])
                    P.op('dve', lambda e, bk=bk, c=c: e.tensor_tensor(out=vn, in0=U[:, c, :], in1=bk[:, 0:128], op=ALU.subtract), r=[kU, bkey], w=['vn'])
                ob, okey = self.bank()
                if c > 0:
                    P.op('pe', lambda e, ob=ob, c=c: e.matmul(ob[:, 0:128], lhsT=QGT[:, c, :], rhs=Sb, start=True, stop=False), r=[kQGT, 'Sb'], w=[okey])
                P.op('pe', lambda e, ob=ob, c=c: e.matmul(ob[:, 0:128], lhsT=AT[:, c, :], rhs=vn, start=(c == 0), stop=True), r=[kAT, 'vn'], w=[okey])
                if c < NT - 1:
                    db_, dkey = self.bank()
                    P.op('pe', lambda e, db_=db_, c=c: e.matmul(db_[:, 0:128], lhsT=kgs[:, c, :], rhs=vn, start=True, stop=True), r=[kkgs, 'vn'], w=[dkey])
                    if c == 0:
                        P.op('dve', lambda e, db_=db_: e.tensor_copy(out=S, in_=db_[:, 0:128]), r=[dkey], w=['S'])
                    else:
                        P.op('dve', lambda e, db_=db_, c=c, h=h: e.scalar_tensor_tensor(out=S, in0=S, scalar=egl[:, c, h:h + 1], in1=db_[:, 0:128],
                                                                                      op0=ALU.mult, op1=ALU.add), r=[dkey, 'S', 'gdg'], w=['S'])
                    P.op('act', lambda e: e.activation(out=Sb, in_=S, func=AF.Copy), r=['S'], w=['Sb'])
                P.op('act', lambda e, ob=ob: e.activation(out=osb, in_=ob[:, 0:128], func=AF.Copy), r=[okey], w=['osb'])
                self.rms_gate(osb, 'osb', yst[:, c, :], 'yst', gt, 'gd_g', 1.0, tmp, 'gdtmp')
                yield
            P.op('pool', lambda e: e.tensor_mul(out=yst, in0=yst, in1=zt), r=['yst', 'zt'], w=['yst'])
            P.dma(lambda e, h=h: e.dma_start(out=ymix[:, 1024 + h * 128:1024 + (h + 1) * 128].rearrange('(t p) d -> p t d', p=128), in_=yst),
                  r=['yst'], w=[self.key('ymix')])

        def step(g, n):
            for _ in range(n):
                try:
                    next(g)
                except StopIteration:
                    return True
            return False

        g0 = front(0, 0)
        while not step(g0, 1000):
            pass
        for h in range(8):
            gb = back(h, h % 2)
            gf = front(h + 1, (h + 1) % 2) if h < 7 else None
            done_b = False
            done_f = gf is None
            while not (done_b and done_f):
                if not done_b:
                    done_b = step(gb, 1)
                if not done_f:
                    done_f = step(gf, 4)
        self.end_phase(mk)

    def proj_feat(self, xT, xkey, W, outT, okey, ntok=T, ncols=2048):
        P = self.P
        for nb in range(ncols // 128):
            wb, wk = self.wload(W, nb * 128, 128, KC, cast=('pool' if nb % 2 == 0 else 'dve'))
            for tb in range(max(1, ntok // 512)):
                n = min(512, ntok)
                bk, bkey = self.bank()
                for kc in range(KC):
                    P.op('pe', lambda e, bk=bk, kc=kc, wb=wb, tb=tb, n=n: e.matmul(
                        bk[:, 0:n], lhsT=wb[:, kc, 0:128], rhs=xT[:, kc, tb * 512:tb * 512 + n],
                        start=(kc == 0), stop=(kc == KC - 1)), r=[xkey, wk], w=[bkey])
                P.op('act', lambda e, bk=bk, nb=nb, tb=tb, n=n: e.activation(out=outT[:, nb, tb * 512:tb * 512 + n], in_=bk[:, 0:n], func=AF.Copy),
                     r=[bkey], w=[okey])

    def phase_xa(self, l, xT, xkey, mem, wq, wkv, qT):
        P, sb = self.P, self.sb
        memT = sb.alloc([KC, 256], BF16)
        self.to_featmajor(mem, memT, 'memT', ntt=2)
        self.wstream_init(KC, 128, nbuf=3)
        kT = sb.alloc([16, 256], BF16)
        V = sb.alloc([2, 2048], BF16)
        self.proj_feat(memT, 'memT', wkv, kT, 'xkT', ntok=256, ncols=2048)
        wv = wkv[:, 2048:4096]
        for nb in range(16):
            wb, wk = self.wload(wv, nb * 128, 128, KC, cast=('pool' if nb % 2 == 0 else 'dve'))
            for mt in range(2):
                bk, bkey = self.bank()
                for kc in range(KC):
                    P.op('pe', lambda e, bk=bk, kc=kc, wb=wb, mt=mt: e.matmul(
                        bk[:, 0:128], lhsT=memT[:, kc, mt * 128:(mt + 1) * 128], rhs=wb[:, kc, 0:128], start=(kc == 0), stop=(kc == KC - 1)),
                        r=['memT', wk], w=[bkey])
                P.op('act', lambda e, bk=bk, mt=mt, nb=nb: e.activation(out=V[:, mt, nb * 128:(nb + 1) * 128], in_=bk[:, 0:128], func=AF.Copy),
                     r=[bkey], w=['xV'])
        self.proj_feat(xT, xkey, wq, qT, 'xqT')
        pT = [sb.alloc([2, 512], BF16) for _ in range(2)]
        pk = [self.key('xp') for _ in range(2)]
        rs = sb.alloc([512], F32)
        sc = 512 ** -0.5
        it = 0
        for hd in range(4):
            for tb in range(4):
                p_, pkk = pT[it % 2], pk[it % 2]
                it += 1
                tsl = slice(tb * 512, (tb + 1) * 512)
                for mt in range(2):
                    bk, bkey = self.bank()
                    for dc in range(4):
                        P.op('pe', lambda e, bk=bk, dc=dc, mt=mt, hd=hd, tsl=tsl: e.matmul(
                            bk, lhsT=kT[:, hd * 4 + dc, mt * 128:(mt + 1) * 128], rhs=qT[:, hd * 4 + dc, tsl], start=(dc == 0), stop=(dc == 3)),
                            r=['xkT', 'xqT'], w=[bkey])
                    P.op('act', lambda e, bk=bk, mt=mt, p_=p_: e.activation(out=p_[:, mt, :], in_=bk, func=AF.Exp, scale=sc), r=[bkey], w=[pkk])
                sbk, skey = self.bank()
                for mt in range(2):
                    P.op('pe', lambda e, sbk=sbk, mt=mt, p_=p_: e.matmul(sbk, lhsT=self.ones_b, rhs=p_[:, mt, :], start=(mt == 0), stop=(mt == 1)),
                         r=[pkk, 'cmask'], w=[skey])
                P.op('dve', lambda e, sbk=sbk: e.reciprocal(out=rs, in_=sbk), r=[skey], w=['xrs'])
                for dvc in range(4):
                    bk, bkey = self.bank()
                    for mt in range(2):
                        P.op('pe', lambda e, bk=bk, mt=mt, p_=p_, dvc=dvc, hd=hd: e.matmul(
                            bk, lhsT=V[:, mt, hd * 512 + dvc * 128:hd * 512 + (dvc + 1) * 128], rhs=p_[:, mt, :], start=(mt == 0), stop=(mt == 1)),
                            r=[pkk, 'xV'], w=[bkey])
                    P.op('dve', lambda e, bk=bk, dvc=dvc, hd=hd, tsl=tsl: e.tensor_tensor(out=qT[:, hd * 4 + dvc, tsl], in0=bk, in1=rs, op=ALU.mult),
                         r=[bkey, 'xrs', 'xqT'], w=['xqT'])

    def route_tile(self, tt, hs, hkey, rw, rbias, comb, sm):
        P = self.P
        hT = self.rt_hT
        mxT = self.moe_xT
        for g in range(4):
            bk, bkey = self.bank()
            for j in range(4):
                kc = g * 4 + j
                P.op('pe', lambda e, bk=bk, j=j, kc=kc: e.transpose(out=bk[:, j * 128:(j + 1) * 128], in_=hs[:, kc * 128:(kc + 1) * 128], identity=self.ident),
                     r=[hkey, 'ident'], w=[bkey])
            P.op('act', lambda e, bk=bk, g=g: e.activation(out=hT[:, g * 4:(g + 1) * 4, :], in_=bk.rearrange('p (a b) -> p a b', a=4), func=AF.Copy),
                 r=[bkey], w=['rt_hT'])
            P.op('pool', lambda e, g=g, tt=tt: e.tensor_copy(out=mxT[:, g * 4:(g + 1) * 4, tt * 128:(tt + 1) * 128],
                                                             in_=hT[:, g * 4:(g + 1) * 4, :]), r=['rt_hT'], w=['moe_xT'])
        bk, bkey = self.bank()
        for kc in range(KC):
            P.op('pe', lambda e, bk=bk, kc=kc: e.matmul(bk[:, 0:16], lhsT=hT[:, kc, :], rhs=rw[:, kc, :], start=(kc == 0), stop=(kc == KC - 1)),
                 r=['rt_hT', 'rw'], w=[bkey])
        k = 'rsm'
        s, sel, t1, t2 = sm[:, 0, :], sm[:, 1, :], sm[:, 2, :], sm[:, 3, :]
        m4, m4b, gm = sm[:, 4, 0:4], sm[:, 4, 4:8], sm[:, 4, 8:9]
        e1 = sm[:, 4, 9:10]
        BIG = 1.0e4
        v3 = lambda a: a.rearrange('p (g s) -> p g s', g=4)
        P.op('act', lambda e, bk=bk: e.activation(out=s, in_=bk[:, 0:16], func=AF.Sigmoid), r=[bkey], w=[k])
        P.op('dve', lambda e: e.tensor_add(out=sel, in0=s, in1=rbias), r=[k, 'rbias'], w=[k])
        P.op('dve', lambda e: e.reduce_max(out=m4, in_=v3(sel), axis=AX.X), r=[k], w=[k])
        P.op('dve', lambda e: e.tensor_tensor(out=v3(t1), in0=v3(sel), in1=m4.rearrange('p (g o) -> p g o', o=1).to_broadcast([128, 4, 4]), op=ALU.is_equal), r=[k], w=[k])
        P.op('dve', lambda e: e.scalar_tensor_tensor(out=t1, in0=t1, scalar=-BIG, in1=sel, op0=ALU.mult, op1=ALU.add), r=[k], w=[k])
        P.op('dve', lambda e: e.reduce_max(out=m4b, in_=v3(t1), axis=AX.X), r=[k], w=[k])
        P.op('dve', lambda e: e.tensor_add(out=m4, in0=m4, in1=m4b), r=[k], w=[k])
        P.op('dve', lambda e: e.reduce_max(out=gm, in_=m4, axis=AX.X), r=[k], w=[k])
        P.op('dve', lambda e: e.tensor_scalar(out=m4b, in0=m4, scalar1=gm, scalar2=None, op0=ALU.is_equal), r=[k], w=[k])
        P.op('dve', lambda e: e.tensor_scalar(out=m4b, in0=m4b, scalar1=-1.0, scalar2=BIG, op0=ALU.add, op1=ALU.mult), r=[k], w=[k])
        P.op('dve', lambda e: e.tensor_tensor(out=v3(t1), in0=v3(sel), in1=m4b.rearrange('p (g o) -> p g o', o=1).to_broadcast([128, 4, 4]), op=ALU.add), r=[k], w=[k])
        P.op('dve', lambda e: e.reduce_max(out=e1, in_=t1, axis=AX.X), r=[k], w=[k])
        P.op('dve', lambda e: e.tensor_scalar(out=t2, in0=t1, scalar1=e1, scalar2=None, op0=ALU.is_equal), r=[k], w=[k])
        P.op('dve', lambda e: e.scalar_tensor_tensor(out=t1, in0=t2, scalar=-BIG, in1=t1, op0=ALU.mult, op1=ALU.add), r=[k], w=[k])
        P.op('dve', lambda e: e.reduce_max(out=e1, in_=t1, axis=AX.X), r=[k], w=[k])
        P.op('dve', lambda e: e.tensor_scalar(out=t1, in0=t1, scalar1=e1, scalar2=None, op0=ALU.is_equal), r=[k], w=[k])
        P.op('dve', lambda e: e.tensor_add(out=t1, in0=t1, in1=t2), r=[k], w=[k])
        P.op('dve', lambda e: e.tensor_mul(out=t1, in0=t1, in1=s), r=[k], w=[k])
        P.op('dve', lambda e: e.reduce_sum(out=e1, in_=t1, axis=AX.X), r=[k], w=[k])
        P.op('dve', lambda e: e.reciprocal(out=e1, in_=e1), r=[k], w=[k])
        P.op('dve', lambda e, tt=tt: e.tensor_scalar_mul(out=comb[:, tt, :], in0=t1, scalar1=e1), r=[k], w=['comb'])

    def phase_moe(self, l, hsrc, hdst, router_w, router_bias, w_in, w_out, ln_g_row, ln_b_row):
        P, sb = self.P, self.sb
        mk = sb.mark()
        comb = sb.alloc([16, 16], F32)
        rw = sb.alloc([KC, 16], F32)
        rbias = sb.alloc([16], F32)
        sm = sb.alloc([5, 16], F32)
        P.dma(lambda e: e.dma_start(out=rw, in_=router_w.rearrange('(k p) n -> p k n', p=128)), w=['rw'])
        P.dma(lambda e: e.dma_start(out=rbias, in_=router_bias.partition_broadcast(128)), w=['rbias'])
        g, b, pk = self.ln_params(ln_g_row, ln_b_row)
        xT = sb.alloc([KC, 1024], BF16)
        self.moe_xT = xT
        acc = sb.alloc([8, 2048], F32)
        actT = sb.alloc([8, 1024], BF16)
        sl = sb.alloc([512], F32)
        hs2 = [sb.alloc([2048], F32) for _ in range(2)]
        hk2 = [self.key('mhs') for _ in range(2)]
        mk2 = sb.mark()
        for hf in range(2):
            self.rt_hT = sb.alloc([KC, 128], F32)
            for t8 in range(8):
                tt = hf * 8 + t8
                hs, hkk = hs2[t8 % 2], hk2[t8 % 2]
                P.dma(lambda e, hs=hs, tt=tt: e.dma_start(out=hs, in_=hsrc[tt * 128:(tt + 1) * 128, :]), w=[hkk])
                self.route_tile(t8, hs, hkk, rw, rbias, comb[:, hf * 8:(hf + 1) * 8, :], sm)
            sb.release(mk2)
            P.barrier()
            self.wstream_init(KC, 256, nbuf=4)
            wo_bf = [sb.alloc([8, 256], BF16) for _ in range(3)]
            wo_bk = [self.key('wo_b') for _ in range(3)]
            woi = 0
            for ex in range(16):
                wi = w_in[ex]
                for fcp in range(4):
                    wg, wgk = self.wload(wi, fcp * 256, 256, KC)
                    wu, wuk = self.wload(wi, 1024 + fcp * 256, 256, KC)
                    for j2 in range(2):
                        fc = fcp * 2 + j2
                        for tb in range(2):
                            tsl = slice(tb * 512, (tb + 1) * 512)
                            gb_, gkey = self.bank()
                            ub_, ukey = self.bank()
                            for kc in range(KC):
                                P.op('pe', lambda e, gb_=gb_, kc=kc, wg=wg, tsl=tsl, j2=j2: e.matmul(gb_, lhsT=wg[:, kc, j2 * 128:(j2 + 1) * 128], rhs=xT[:, kc, tsl],
                                                                                                  start=(kc == 0), stop=(kc == KC - 1)), r=['moe_xT', wgk], w=[gkey])
                            for kc in range(KC):
                                P.op('pe', lambda e, ub_=ub_, kc=kc, wu=wu, tsl=tsl, j2=j2: e.matmul(ub_, lhsT=wu[:, kc, j2 * 128:(j2 + 1) * 128], rhs=xT[:, kc, tsl],
                                                                                                  start=(kc == 0), stop=(kc == KC - 1)), r=['moe_xT', wuk], w=[ukey])
                            P.op('act', lambda e, gb_=gb_: e.activation(out=sl, in_=gb_, func=AF.Silu), r=[gkey], w=['sl'])
                            P.op('dve', lambda e, ub_=ub_, fc=fc, tsl=tsl: e.tensor_tensor(out=actT[:, fc, tsl], in0=sl, in1=ub_, op=ALU.mult),
                                 r=['sl', ukey], w=['actT'])
                wo = w_out[ex].rearrange('(k p) n -> p k n', p=128)
                for cb in range(8):
                    i = woi % 3
                    woi += 1
                    P.dma(lambda e, i=i, cb=cb, wo=wo: e.dma_start(out=wo_bf[i], in_=wo[:, :, cb * 256:(cb + 1) * 256]), w=[wo_bk[i]], q='pool')
                    for t8 in range(8):
                        bk, bkey = self.bank()
                        for fc in range(8):
                            P.op('pe', lambda e, bk=bk, fc=fc, t8=t8, i=i: e.matmul(bk[:, 0:256], lhsT=actT[:, fc, t8 * 128:(t8 + 1) * 128], rhs=wo_bf[i][:, fc, :],
                                                                                 start=(fc == 0), stop=(fc == 7)), r=['actT', wo_bk[i]], w=[bkey])
                        a_ = acc[:, t8, cb * 256:(cb + 1) * 256]
                        cs_ = comb[:, hf * 8 + t8, ex:ex + 1]
                        if ex == 0:
                            P.op('dve', lambda e, bk=bk, a_=a_, cs_=cs_: e.tensor_scalar_mul(out=a_, in0=bk[:, 0:256], scalar1=cs_), r=[bkey, 'comb'], w=['acc'])
                        else:
                            P.op('dve', lambda e, bk=bk, a_=a_, cs_=cs_: e.scalar_tensor_tensor(out=a_, in0=bk[:, 0:256], scalar=cs_, in1=a_, op0=ALU.mult, op1=ALU.add),
                                 r=[bkey, 'comb', 'acc'], w=['acc'])
            for t8 in range(8):
                tt = hf * 8 + t8
                hs, hkk = hs2[t8 % 2], hk2[t8 % 2]
                P.dma(lambda e, hs=hs, tt=tt: e.dma_start(out=hs, in_=hsrc[tt * 128:(tt + 1) * 128, :]), w=[hkk])
                a_ = acc[:, t8, :]
                P.op('dve', lambda e, a_=a_, hs=hs: e.scalar_tensor_tensor(out=a_, in0=hs, scalar=ALPHA, in1=a_, op0=ALU.mult, op1=ALU.add),
                     r=[hkk, 'acc'], w=['acc'])
                self.ln_tile(a_, 'acc', g, b, pk, hs, hkk)
                P.dma(lambda e, hs=hs, tt=tt: e.dma_start(out=hdst[tt * 128:(tt + 1) * 128, :], in_=hs), r=[hkk], w=[self.key('hdst')])
            sb.release(mk2)
            P.barrier()
        self.end_phase(mk)
    def phase_ln(self, lin, hsrc, hdst, ln_g_row, ln_b_row):
        P, sb = self.P, self.sb
        mk = sb.mark()
        g, b, pk = self.ln_params(ln_g_row, ln_b_row)
        ls = [sb.alloc([2048], F32) for _ in range(2)]
        lk = [self.key('lns') for _ in range(2)]
        hs = [sb.alloc([2048], F32) for _ in range(2)]
        hk = [self.key('lnh') for _ in range(2)]
        for tt in range(NT):
            a, ak, h, hkk = ls[tt % 2], lk[tt % 2], hs[tt % 2], hk[tt % 2]
            P.dma(lambda e, a=a, tt=tt: e.dma_start(out=a, in_=lin[tt * 128:(tt + 1) * 128, :]), w=[ak])
            P.dma(lambda e, h=h, tt=tt: e.dma_start(out=h, in_=hsrc[tt * 128:(tt + 1) * 128, :]), w=[hkk])
            P.op('dve', lambda e, a=a, h=h: e.scalar_tensor_tensor(out=a, in0=h, scalar=ALPHA, in1=a, op0=ALU.mult, op1=ALU.add),
                 r=[hkk, ak], w=[ak])
            self.ln_tile(a, ak, g, b, pk, h, hkk)
            P.dma(lambda e, h=h, tt=tt: e.dma_start(out=hdst[tt * 128:(tt + 1) * 128, :], in_=h), r=[hkk], w=[self.key('hdst')])
        self.end_phase(mk)


SB_BYTES = 204800


def build_nc(stop_after=None, dbg=False):
    import math
    import contextlib
    nc = bass.Bass("TRN2", target_bir_lowering=False)
    dr = {}

    def inp(name, shape):
        dr[name] = nc.dram_tensor(name, list(shape), F32, kind="ExternalInput").ap()

    inp('x', (T, D)); inp('mem', (256, D)); inp('w_in', (DEPTH, D, IN_W)); inp('da_lambda', (DEPTH, 4, 64))
    inp('da_norm_g', (DEPTH, 128)); inp('ml_gate_b', (DEPTH, 2, 4)); inp('ml_norm_g', (DEPTH, 128))
    inp('gd_conv_w', (DEPTH, 4, 3072)); inp('gd_a_log', (DEPTH, 8)); inp('gd_dt_bias', (DEPTH, 8)); inp('gd_norm_g', (DEPTH, 128))
    inp('w_out', (DEPTH, D, D)); inp('xa_wq', (DEPTH, D, D)); inp('xa_wkv', (DEPTH, D, 2 * D)); inp('xa_wo', (DEPTH, D, D))
    inp('router_w', (D, 16)); inp('router_bias', (16,)); inp('moe_w_in', (DEPTH, 16, D, D)); inp('moe_w_out', (DEPTH, 16, D // 2, D))
    inp('ln_g', (DEPTH, 3, D)); inp('ln_b', (DEPTH, 3, D))
    inp('ident', (128, 128)); inp('cos', (T, 32)); inp('sin', (T, 32))
    y = nc.dram_tensor('y', [T, D], F32, kind="ExternalOutput").ap()
    okind = "ExternalOutput" if dbg else "Internal"
    proj = nc.dram_tensor('proj', [T, IN_W], F32, kind=okind).ap()
    ymix = nc.dram_tensor('ymix', [T, D], F32, kind=okind).ap()
    lin = nc.dram_tensor('lin', [T, D], F32, kind=okind).ap()
    hres = nc.dram_tensor('hres', [T, D], F32, kind=okind).ap()
    with contextlib.ExitStack() as es:
        pool = es.enter_context(nc.sbuf_tensor("pool", [128, SB_BYTES // 4], F32))
        ps = es.enter_context(nc.psum_tensor("ps", [128, 4096], F32))
        P = Prog(nc)
        sb = SB(pool, SB_BYTES)
        m = MK(nc, P, sb, ps, dr)
        m.consts()
        P.barrier()

        def body():
            hsrc = dr['x']
            for l in range(DEPTH):
                lam_init = 0.8 - 0.6 * math.exp(-0.3 * l)
                mk = sb.mark()
                xT = sb.alloc([KC, T], BF16)
                m.to_featmajor(hsrc, xT, 'xT')
                m.phase_proj(xT, 'xT', dr['w_in'][l], proj, IN_W)
                m.end_phase(mk)
                if stop_after == ('proj', l):
                    return
                mk = sb.mark()
                gens = [m.phase_da(l, proj, ymix, dr['da_lambda'][l], dr['da_norm_g'][l], lam_init),
                        m.phase_ml(l, proj, ymix, dr['ml_gate_b'][l], dr['ml_norm_g'][l])]
                while gens:
                    for g_ in list(gens):
                        try:
                            next(g_)
                        except StopIteration:
                            gens.remove(g_)
                m.end_phase(mk)
                if stop_after == ('ml', l):
                    return
                m.phase_gd(l, proj, ymix, dr['gd_conv_w'][l], dr['gd_a_log'][l], dr['gd_dt_bias'][l], dr['gd_norm_g'][l])
                if stop_after == ('gd', l):
                    return
                mk = sb.mark()
                yT = sb.alloc([KC, T], BF16)
                m.to_featmajor(ymix, yT, 'yT')
                m.phase_proj(yT, 'yT', dr['w_out'][l], lin, D)
                m.end_phase(mk)
                m.phase_ln(lin, hsrc, hres, dr['ln_g'][l, 0], dr['ln_b'][l, 0])
                hsrc = hres
                if stop_after == ('mix', l):
                    return
                mk = sb.mark()
                qT = sb.alloc([KC, T], BF16)
                mk1 = sb.mark()
                xT = sb.alloc([KC, T], BF16)
                m.to_featmajor(hres, xT, 'xT')
                m.phase_xa(l, xT, 'xT', dr['mem'], dr['xa_wq'][l], dr['xa_wkv'][l], qT)
                m.end_phase(mk1)
                m.phase_proj(qT, 'xqT', dr['xa_wo'][l], lin, D)
                m.end_phase(mk)
                m.phase_ln(lin, hres, hres, dr['ln_g'][l, 1], dr['ln_b'][l, 1])
                if stop_after == ('xa', l):
                    return
                hdst = y if l == DEPTH - 1 else hres
                m.phase_moe(l, hres, hdst, dr['router_w'], dr['router_bias'], dr['moe_w_in'][l], dr['moe_w_out'][l],
                            dr['ln_g'][l, 2], dr['ln_b'][l, 2])
                if stop_after == ('moe', l):
                    return

        body()
        P.emit()
    return nc


def host_consts():
    inv = 1.0 / (10000.0 ** (np.arange(0, 64, 2, dtype=np.float32) / 64))
    ang = np.arange(T, dtype=np.float32)[:, None] * inv[None, :].astype(np.float32)
    return {'ident': np.eye(128, dtype=np.float32), 'cos': np.cos(ang).astype(np.float32), 'sin': np.sin(ang).astype(np.float32)}


_NC_CACHE = {}


def kernel(**inputs):
    if 'nc' not in _NC_CACHE:
        _NC_CACHE['nc'] = build_nc()
    nc = _NC_CACHE['nc']
    hc = host_consts()
    shared = {k: np.ascontiguousarray(np.asarray(v, dtype=np.float32)) for k, v in inputs.items() if k not in ('x', 'mem')}
    shared.update(hc)
    x = np.asarray(inputs['x'], dtype=np.float32)
    mem = np.asarray(inputs['mem'], dtype=np.float32)
    in_maps = []
    for c in range(8):
        d = dict(shared)
        d['x'] = np.ascontiguousarray(x[c])
        d['mem'] = np.ascontiguousarray(mem[c])
        in_maps.append(d)
    res = run_bass_kernel_spmd(nc, in_maps, core_ids=list(range(8)))
    return np.stack([np.asarray(r['y']) for r in res.results], axis=0).astype(np.float32)
```
